# Optimizing a Trainium2 kernel written in Bass

```python
import math
import jax, jax.numpy as jnp
from jax import lax
import numpy as np

D_MODEL = 1024
BATCH = 4
SEQ = 8192
DEPTH = 1

CHUNK = 64
GM_WIDTH = 512
GM_GROUPS = 8
GM_BLOCK = 128
FOX_HEADS = 8
FOX_HEAD_DIM = 64
FOX_WIDTH = FOX_HEADS * FOX_HEAD_DIM
FOX_Q_BLOCK = 128
MEM_LEN = 256
MEM_HEADS = 4
MEM_HEAD_DIM = D_MODEL // MEM_HEADS
N_EXPERTS = 32
TOP_K = 4
D_EXPERT = D_MODEL
SWIGLU_ALPHA = 1.702
SWIGLU_LIMIT = 7.0
MOE_BLOCK = 512
LN_EPS = 1e-5
DEEPNORM_ALPHA = (2 * DEPTH) ** 0.25
DEEPNORM_BETA = (8 * DEPTH) ** -0.25
IN_SIZES = (GM_WIDTH, GM_WIDTH, FOX_WIDTH, FOX_WIDTH, FOX_WIDTH, FOX_HEADS, D_MODEL, D_MODEL)
IN_COLS = sum(IN_SIZES)

kernel_name = "hybrid_gmlp_fox_memxattn_moe_deepnorm"


def layer_norm(x, g, b):
    xf = x.astype(jnp.float32)
    mu = jnp.mean(xf, axis=-1, keepdims=True)
    var = jnp.mean(jnp.square(xf - mu), axis=-1, keepdims=True)
    return ((xf - mu) * lax.rsqrt(var + LN_EPS) * g + b).astype(x.dtype)


def spatial_gating(u, v, ln_v_g, ln_v_b, w_sp, b_sp):
    B_, S_, _ = v.shape
    v = layer_norm(v, ln_v_g, ln_v_b)
    pos = jnp.arange(GM_BLOCK)
    chunk_mask = (pos[None, :] // CHUNK) <= (pos[:, None] // CHUNK)
    w = jnp.where(chunk_mask[None], w_sp, 0)
    vb = v.reshape(B_, S_ // GM_BLOCK, GM_BLOCK, GM_GROUPS, GM_WIDTH // GM_GROUPS)
    mixed = jnp.einsum('gts,bnsgc->bntgc', w, vb) + b_sp.T[None, None, :, :, None]
    return u * mixed.reshape(B_, S_, GM_WIDTH)


def forgetting_attention(q, k, v, log_f):
    S_ = q.shape[2]
    c = jnp.cumsum(log_f, axis=-1)
    scale = FOX_HEAD_DIM ** -0.5
    outs = []
    for i in range(S_ // FOX_Q_BLOCK):
        q0, q1 = i * FOX_Q_BLOCK, (i + 1) * FOX_Q_BLOCK
        logits = jnp.einsum('bhqd,bhkd->bhqk', q[:, :, q0:q1], k[:, :, :q1]).astype(jnp.float32) * scale
        logits = logits + c[:, :, q0:q1, None] - c[:, :, None, :q1]
        causal = jnp.arange(q0, q1)[:, None] >= jnp.arange(q1)[None, :]
        p = jax.nn.softmax(jnp.where(causal, logits, -jnp.inf), axis=-1)
        outs.append(jnp.einsum('bhqk,bhkd->bhqd', p.astype(v.dtype), v[:, :, :q1]))
    return jnp.concatenate(outs, axis=2)


def memory_cross_attention(x, mem, w_mq, w_mkv, w_mo):
    B_, S_, _ = x.shape
    M_ = mem.shape[1]
    q = (x @ w_mq).reshape(B_, S_, MEM_HEADS, MEM_HEAD_DIM)
    kv = (mem @ w_mkv).reshape(B_, M_, 2, MEM_HEADS, MEM_HEAD_DIM)
    k, v = kv[:, :, 0], kv[:, :, 1]
    logits = jnp.einsum('bshd,bmhd->bhsm', q, k).astype(jnp.float32) * (MEM_HEAD_DIM ** -0.5)
    p = jax.nn.softmax(logits, axis=-1)
    o = jnp.einsum('bhsm,bmhd->bshd', p.astype(v.dtype), v).reshape(B_, S_, D_MODEL)
    return o @ w_mo


def moe_ffn(x, w_router, b_router, w_gate_up, b_gate_up, w_down, b_down):
    B_, S_, D_ = x.shape
    xt = x.reshape(-1, D_)
    T = xt.shape[0]
    logits = (xt @ w_router + b_router).astype(jnp.float32)
    top_val, top_idx = lax.top_k(logits, TOP_K)
    gate_w = jax.nn.softmax(top_val, axis=-1)
    A = T * TOP_K
    e_flat = top_idx.reshape(-1)
    tok_flat = jnp.arange(A, dtype=jnp.int32) // TOP_K
    w_flat = gate_w.reshape(-1)
    order = jnp.argsort(e_flat)
    e_sorted = e_flat[order]
    counts = jnp.bincount(e_flat, length=N_EXPERTS)
    padded = (counts + MOE_BLOCK - 1) // MOE_BLOCK * MOE_BLOCK
    start_sorted = jnp.cumsum(counts) - counts
    end_padded = jnp.cumsum(padded)
    start_padded = end_padded - padded
    dest = start_padded[e_sorted] + jnp.arange(A, dtype=jnp.int32) - start_sorted[e_sorted]
    n_blocks = -(-(A + N_EXPERTS * (MOE_BLOCK - 1)) // MOE_BLOCK)
    P = n_blocks * MOE_BLOCK
    tok_pad = jnp.zeros((P,), jnp.int32).at[dest].set(tok_flat[order])
    w_pad = jnp.zeros((P,), jnp.float32).at[dest].set(w_flat[order])
    block_start = jnp.arange(n_blocks, dtype=jnp.int32) * MOE_BLOCK
    block_e = jnp.minimum(jnp.searchsorted(end_padded, block_start, side='right'), N_EXPERTS - 1)

    def expert_block(args):
        tok, e = args
        h = xt[tok] @ w_gate_up[e] + b_gate_up[e]
        gate, up = h[:, :D_EXPERT], h[:, D_EXPERT:]
        gate = jnp.minimum(gate, SWIGLU_LIMIT)
        up = jnp.clip(up, -SWIGLU_LIMIT, SWIGLU_LIMIT)
        glu = gate * jax.nn.sigmoid(SWIGLU_ALPHA * gate)
        return ((up + 1) * glu) @ w_down[e] + b_down[e]

    y = lax.map(expert_block, (tok_pad.reshape(n_blocks, MOE_BLOCK), block_e))
    y = y.reshape(P, D_) * w_pad[:, None].astype(y.dtype)
    out = jax.ops.segment_sum(y, tok_pad, num_segments=T)
    return out.reshape(B_, S_, D_)


def setup_inputs(seed: int = 0) -> dict:
    key = jax.random.key(seed)
    ks = jax.random.split(key, 32)
    L = DEPTH

    def nrm(k, shape, scale):
        return jax.random.normal(k, shape, jnp.float32) * scale

    def gain(k, shape):
        return 1.0 + 0.05 * jax.random.normal(k, shape, jnp.float32)

    return {
        "x": nrm(ks[0], (BATCH, SEQ, D_MODEL), 1.0),
        "mem": nrm(ks[1], (BATCH, MEM_LEN, D_MODEL), 1.0),
        "ln_in_g": gain(ks[2], (D_MODEL,)),
        "ln_in_b": nrm(ks[3], (D_MODEL,), 0.02),
        "w_in": nrm(ks[4], (L, D_MODEL, IN_COLS), D_MODEL ** -0.5),
        "b_forget": 2.0 + nrm(ks[5], (L, FOX_HEADS), 0.5),
        "ln_v_g": gain(ks[6], (L, GM_WIDTH)),
        "ln_v_b": nrm(ks[7], (L, GM_WIDTH), 0.02),
        "w_spatial": nrm(ks[8], (L, GM_GROUPS, GM_BLOCK, GM_BLOCK), GM_BLOCK ** -0.5),
        "b_spatial": 1.0 + nrm(ks[9], (L, GM_GROUPS, GM_BLOCK), 0.1),
        "w_branch_a": nrm(ks[10], (L, GM_WIDTH, D_MODEL), GM_WIDTH ** -0.5),
        "w_branch_b": nrm(ks[11], (L, FOX_WIDTH, D_MODEL), FOX_WIDTH ** -0.5),
        "w_out": nrm(ks[12], (L, D_MODEL, D_MODEL), DEEPNORM_BETA * D_MODEL ** -0.5),
        "ln1_g": gain(ks[13], (L, D_MODEL)),
        "ln1_b": nrm(ks[14], (L, D_MODEL), 0.02),
        "ln_mem_g": gain(ks[15], (L, D_MODEL)),
        "ln_mem_b": nrm(ks[16], (L, D_MODEL), 0.02),
        "w_mq": nrm(ks[17], (L, D_MODEL, D_MODEL), D_MODEL ** -0.5),
        "w_mkv": nrm(ks[18], (L, D_MODEL, 2 * D_MODEL), D_MODEL ** -0.5),
        "w_mo": nrm(ks[19], (L, D_MODEL, D_MODEL), DEEPNORM_BETA * D_MODEL ** -0.5),
        "ln2_g": gain(ks[20], (L, D_MODEL)),
        "ln2_b": nrm(ks[21], (L, D_MODEL), 0.02),
        "w_router": nrm(ks[22], (L, D_MODEL, N_EXPERTS), D_MODEL ** -0.5),
        "b_router": nrm(ks[23], (L, N_EXPERTS), 0.01),
        "w_gate_up": nrm(ks[24], (L, N_EXPERTS, D_MODEL, 2 * D_EXPERT), D_MODEL ** -0.5),
        "b_gate_up": nrm(ks[25], (L, N_EXPERTS, 2 * D_EXPERT), 0.01),
        "w_down": nrm(ks[26], (L, N_EXPERTS, D_EXPERT, D_MODEL), DEEPNORM_BETA * D_EXPERT ** -0.5),
        "b_down": nrm(ks[27], (L, N_EXPERTS, D_MODEL), 0.01),
        "ln3_g": gain(ks[28], (L, D_MODEL)),
        "ln3_b": nrm(ks[29], (L, D_MODEL), 0.02),
    }


def reference(x, mem, ln_in_g, ln_in_b, w_in, b_forget, ln_v_g, ln_v_b, w_spatial, b_spatial,
              w_branch_a, w_branch_b, w_out, ln1_g, ln1_b, ln_mem_g, ln_mem_b, w_mq, w_mkv, w_mo,
              ln2_g, ln2_b, w_router, b_router, w_gate_up, b_gate_up, w_down, b_down, ln3_g, ln3_b):
    B_, S_, _ = x.shape
    split_at = [int(i) for i in np.cumsum(IN_SIZES)[:-1]]
    x = layer_norm(x, ln_in_g, ln_in_b)
    for l in range(DEPTH):
        h = x @ w_in[l]
        u_gm, v_gm, q, k, v, f_logit, g_a, g_b = jnp.split(h, split_at, axis=-1)
        y_a = spatial_gating(jax.nn.gelu(u_gm, approximate=False), jax.nn.gelu(v_gm, approximate=False),
                             ln_v_g[l], ln_v_b[l], w_spatial[l], b_spatial[l])
        to_heads = lambda t: t.reshape(B_, S_, FOX_HEADS, FOX_HEAD_DIM).transpose(0, 2, 1, 3)
        log_f = jax.nn.log_sigmoid((f_logit + b_forget[l]).astype(jnp.float32)).transpose(0, 2, 1)
        y_b = forgetting_attention(to_heads(q), to_heads(k), to_heads(v), log_f)
        y_b = y_b.transpose(0, 2, 1, 3).reshape(B_, S_, FOX_WIDTH)
        merged = jax.nn.sigmoid(g_a) * (y_a @ w_branch_a[l]) + jax.nn.sigmoid(g_b) * (y_b @ w_branch_b[l])
        x = layer_norm(DEEPNORM_ALPHA * x + merged @ w_out[l], ln1_g[l], ln1_b[l])
        mem_n = layer_norm(mem, ln_mem_g[l], ln_mem_b[l])
        x = layer_norm(DEEPNORM_ALPHA * x + memory_cross_attention(x, mem_n, w_mq[l], w_mkv[l], w_mo[l]),
                       ln2_g[l], ln2_b[l])
        y_moe = moe_ffn(x, w_router[l], b_router[l], w_gate_up[l], b_gate_up[l], w_down[l], b_down[l])
        x = layer_norm(DEEPNORM_ALPHA * x + y_moe, ln3_g[l], ln3_b[l])
    return x
```

```python
import os
from contextlib import ExitStack
import numpy as np
import ml_dtypes
import concourse.bass as bass
import concourse.mybir as mybir
from concourse.bass_utils import run_bass_kernel_spmd

F32 = mybir.dt.float32
BF16 = mybir.dt.bfloat16
I32 = mybir.dt.int32
AF = mybir.ActivationFunctionType
ALU = mybir.AluOpType
AX = mybir.AxisListType

D = 1024
SEQ = 8192
NB = 64
NOB = 32
NG = 8
NE = 32
CAP = 896
NSLOT = NE * CAP
ALPHA = 2.0 ** 0.25
EPS = 1e-5
NEG = -30000.0
BIGIDX = 1.0e6

ENGS = ("pe", "act", "dve", "pool", "sp")


class _Op:
    __slots__ = ("eng", "fn", "deps", "odeps", "dma", "inc", "val", "dur", "lat", "idx", "t0", "t1", "nd", "succ")


def _fsz(ap):
    n = 1
    for d in ap.shape[1:]:
        n *= int(d)
    return n


class Prog:
    def __init__(self, nc, esems, dsems):
        self.nc = nc
        self.esems = esems
        self.dsems = dsems
        self.ecnt = {e: 0 for e in ENGS}
        self.dcnt = [0] * len(dsems)
        self.latadd = 0.2
        self.reset_phase()

    def reset_phase(self):
        self.ops = {e: [] for e in ENGS}
        self.allops = []
        self.lastw = {}
        self.rdall = {}
        self.dkey = {}
        self.lastdma = {}

    def add(self, eng, fn, reads=(), writes=(), dma=None, dur=0.3, lat=None):
        op = _Op()
        op.eng = eng
        op.fn = fn
        op.inc = False
        op.val = 0
        op.dma = None
        if dma is not None and lat is None:
            dur, lat = 1.0, 3.5
        op.dur = dur
        op.lat = (dur + self.latadd) if lat is None else lat
        op.odeps = []
        deps = set()
        for k in reads:
            w = self.lastw.get(k)
            if w is not None:
                deps.add(w)
        for k in writes:
            w = self.lastw.get(k)
            if w is not None:
                deps.add(w)
            for r in self.rdall.get(k, ()):
                deps.add(r)
        op.deps = deps
        if dma is not None:
            if dma not in self.dkey:
                self.dkey[dma] = len(self.dkey)
                assert len(self.dkey) <= len(self.dsems), "too many dma keys"
            si = self.dkey[dma]
            self.dcnt[si] += 16
            op.dma = si
            op.val = self.dcnt[si]
            prev = self.lastdma.get(si)
            if prev is not None:
                assert prev.eng == eng, "dma key shared between engines"
                op.odeps.append(prev)
            self.lastdma[si] = op
        for k in reads:
            self.rdall.setdefault(k, []).append(op)
        for k in writes:
            self.lastw[k] = op
            self.rdall[k] = []
        op.idx = len(self.allops)
        self.allops.append(op)
        self.ops[eng].append(op)
        return op

    def schedule(self):
        import heapq
        ops = self.allops
        for op in ops:
            op.succ = []
            op.nd = 0
        for op in ops:
            for d in op.deps:
                d.succ.append((op, 0))
                op.nd += 1
            for d in op.odeps:
                if d not in op.deps:
                    d.succ.append((op, 1))
                    op.nd += 1
        import bisect
        rt = [0.0] * len(ops)
        cand = {e: [] for e in ENGS}
        free = {e: 0.0 for e in ENGS}
        for op in ops:
            if op.nd == 0:
                cand[op.eng].append(op.idx)
        order = {e: [] for e in ENGS}
        W = 24
        remaining = len(ops)
        while remaining:
            best = None
            for e in ENGS:
                h = cand[e]
                if not h:
                    continue
                fe = free[e] + 0.05
                ch = None
                n = min(W, len(h))
                for pos in range(n):
                    ix = h[pos]
                    if rt[ix] <= fe:
                        ch = (free[e], ix, pos)
                        break
                if ch is None:
                    pos = min(range(n), key=lambda p_: (rt[h[p_]], p_))
                    ch = (rt[h[pos]], h[pos], pos)
                if best is None or ch[:2] < best[0][:2]:
                    best = (ch, e)
            (st, ix, pos), e = best
            op = ops[ix]
            del cand[e][pos]
            st = max(st, free[e])
            op.t0 = st
            op.t1 = st + op.lat
            free[e] = st + op.dur
            order[e].append(op)
            remaining -= 1
            for (sx, kind) in op.succ:
                tdep = op.t0 if kind == 1 else op.t1
                if tdep > rt[sx.idx]:
                    rt[sx.idx] = tdep
                sx.nd -= 1
                if sx.nd == 0:
                    bisect.insort(cand[sx.eng], sx.idx)
        self.ops = order
        self.sim_time = max(free.values())
        if os.environ.get("KVERBOSE") == "1":
            busy = {e: sum(o.dur for o in order[e]) for e in ENGS}
            print("phase sim_time us %.0f nops %d" % (self.sim_time, len(ops)), " ".join("%s=%.0f" % (e, busy[e]) for e in ENGS))

    def emit(self):
        nc = self.nc
        if os.environ.get("KNOSCHED") != "1":
            self.schedule()
        for e in ENGS:
            for op in self.ops[e]:
                for d in op.deps:
                    if d.dma is None and not (d.eng == "pe" and op.eng == "pe"):
                        d.inc = True
        for e in ENGS:
            for op in self.ops[e]:
                if op.dma is None and op.inc:
                    self.ecnt[e] += 1
                    op.val = self.ecnt[e]
        final_d = list(self.dcnt)
        used_d = sorted(set(self.dkey.values()))

        def run(ename, eng):
            known = {}
            for op in self.ops[ename]:
                need = {}
                for d in op.deps:
                    if d.dma is not None:
                        key = ("d", d.dma)
                    else:
                        if d.eng == "pe" and ename == "pe":
                            continue
                        key = ("e", d.eng)
                    if d.val > need.get(key, 0):
                        need[key] = d.val
                for key, val in need.items():
                    if known.get(key, 0) >= val:
                        continue
                    known[key] = val
                    sem = self.dsems[key[1]] if key[0] == "d" else self.esems[key[1]]
                    eng.wait_ge(sem, val)
                inst = op.fn(eng)
                if op.dma is not None:
                    inst.then_inc(self.dsems[op.dma], 16)
                elif op.inc:
                    inst.then_inc(self.esems[ename], 1)
            if ename == "sp":
                for si in used_d:
                    eng.wait_ge(self.dsems[si], final_d[si])

        with nc.Block() as block:
            @block.tensor
            def _(e):
                run("pe", e)

            @block.scalar
            def _(e):
                run("act", e)

            @block.vector
            def _(e):
                run("dve", e)

            @block.gpsimd
            def _(e):
                run("pool", e)

            @block.sync
            def _(e):
                run("sp", e)
        nc.all_engine_barrier()
        self.reset_phase()

    @staticmethod
    def _edur(eng, ap):
        f = _fsz(ap)
        if eng == "act":
            return 0.22 + f / 1400.0
        if eng == "dve":
            return 0.12 + f / 960.0
        return 0.3 + f / 420.0

    def mm(self, out, lhsT, rhs, start, stop, r, w):
        self.add("pe", lambda e: e.matmul(out, lhsT=lhsT, rhs=rhs, start=start, stop=stop,
                                          skip_group_check=True), r, w, dur=0.03 + _fsz(rhs) / 2300.0)

    def tr(self, out, in_, ident, r, w):
        self.add("pe", lambda e: e.transpose(out, in_, ident), r, w, dur=0.14)

    def actf(self, out, in_, func, r, w, bias=None, scale=None, eng="act"):
        kw = {}
        if bias is not None:
            kw["bias"] = bias
        if scale is not None:
            kw["scale"] = scale
        self.add(eng, lambda e: e.activation(out=out, in_=in_, func=func, **kw), r, w, dur=self._edur(eng, out))

    def copy(self, eng, out, in_, r, w):
        if eng == "act":
            self.add(eng, lambda e: e.activation(out=out, in_=in_, func=AF.Copy), r, w, dur=self._edur(eng, out))
        else:
            self.add(eng, lambda e: e.tensor_copy(out=out, in_=in_), r, w, dur=self._edur(eng, out))

    def tt(self, eng, out, in0, in1, op, r, w):
        self.add(eng, lambda e: e.tensor_tensor(out=out, in0=in0, in1=in1, op=op), r, w, dur=self._edur(eng, out))

    def ts(self, eng, out, in0, s1, s2, op0, op1, r, w):
        if op1 is None:
            self.add(eng, lambda e: e.tensor_scalar(out=out, in0=in0, scalar1=s1, scalar2=None, op0=op0), r, w,
                     dur=self._edur(eng, out))
        else:
            self.add(eng, lambda e: e.tensor_scalar(out=out, in0=in0, scalar1=s1, scalar2=s2,
                                                    op0=op0, op1=op1), r, w, dur=self._edur(eng, out))

    def stt(self, eng, out, in0, scalar, in1, op0, op1, r, w):
        self.add(eng, lambda e: e.scalar_tensor_tensor(out=out, in0=in0, scalar=scalar, in1=in1,
                                                       op0=op0, op1=op1), r, w, dur=self._edur(eng, out))

    def memset(self, eng, ap, val, w):
        self.add(eng, lambda e: e.memset(ap, val), (), w, dur=self._edur(eng, ap))

    def dma(self, eng, out, in_, key, r, w):
        nbytes = _fsz(out) * int(out.shape[0]) * (2 if out.dtype == BF16 else 4)
        self.add(eng, lambda e: e.dma_start(out=out, in_=in_), r, w, dma=key,
                 dur=(1.0 if eng == "pool" else 0.1), lat=2.2 + nbytes / 120e3)

    def layernorm(self, x, o, g, b, width, tmp, xk, ok, gk, tk, eng2="dve"):
        self.lncnt = getattr(self, "lncnt", 0) + 1
        si = self.lncnt % len(tmp)
        tk = tk + str(si)
        eps = tmp[0]["eps"]
        tmp = tmp[si]
        st, mv, rstd = tmp["st"], tmp["mv"], tmp["rstd"]
        nch = width // 512
        for c in range(nch):
            self.add("dve", (lambda c: lambda e: e.bn_stats(out=st[:, c * 6:(c + 1) * 6],
                                                             in_=x[:, c * 512:(c + 1) * 512]))(c),
                     xk, [tk + "st"])
        self.add("dve", lambda e: e.bn_aggr(out=mv[:, 0:2], in_=st[:, 0:6 * nch]), [tk + "st"], [tk + "mv"])
        lnv, nmr = tmp["lnv"], tmp["nmr"]
        self.actf(lnv[:, 0:1], mv[:, 1:2], AF.Ln, [tk + "mv"], [tk + "lnv"], bias=eps[:, 0:1])
        self.actf(rstd[:, 0:1], lnv[:, 0:1], AF.Exp, [tk + "lnv"], [tk + "rs"], scale=-0.5)
        self.ts("dve", nmr[:, 0:1], mv[:, 0:1], rstd[:, 0:1], -1.0, ALU.mult, ALU.mult, [tk + "mv", tk + "rs"], [tk + "nmr"])
        self.actf(o, x, AF.Identity, list(xk) + [tk + "nmr", tk + "rs"], ok, bias=nmr[:, 0:1], scale=rstd[:, 0:1])
        self.tt(eng2, o, o, g, ALU.mult, list(ok) + list(gk), ok)
        self.tt(eng2, o, o, b, ALU.add, list(ok) + list(gk), ok)


def _consts(parity):
    k = np.arange(128)
    c = {}
    c["ident_f"] = np.eye(128, dtype=np.float32)
    c["ident_b"] = np.eye(128, dtype=np.float32).astype(ml_dtypes.bfloat16)
    c["ut"] = (k[:, None] <= k[None, :]).astype(np.float32)
    c["ones_f"] = np.ones((128, 128), np.float32)
    c["ones_b"] = np.ones((128, 128), np.float32).astype(ml_dtypes.bfloat16)
    c["cmaskT"] = ((k[:, None] // 64) <= (k[None, :] // 64)).astype(np.float32)
    causal = np.where(k[:, None] <= k[None, :], 0.0, NEG).astype(np.float32)
    zeros = np.zeros((128, 128), np.float32)
    full = np.full((128, 128), NEG, np.float32)
    if parity == 0:
        mE, mO = causal, full
    else:
        mE, mO = zeros, causal
    c["maskE"] = mE.astype(ml_dtypes.bfloat16)
    c["maskO"] = mO.astype(ml_dtypes.bfloat16)
    c["pE"] = np.full((128, 1), 1.0 - parity, np.float32)
    c["pO"] = np.full((128, 1), float(parity), np.float32)
    c["trash"] = np.broadcast_to((NSLOT + np.arange(128)).astype(np.float32)[:, None], (128, NE)).copy()
    c["ecap"] = np.broadcast_to((np.arange(NE) * CAP).astype(np.float32)[None, :], (128, NE)).copy()
    return c


def build_program(debug=False, stop_after=99):
    nc = bass.Bass("TRN2", target_bir_lowering=False)
    ins = {}

    def din(name, shape, dt=F32):
        ins[name] = nc.dram_tensor(name, list(shape), dt, kind="ExternalInput").ap()
        return ins[name]

    skind = "ExternalOutput" if debug else "Internal"

    def dscr(name, shape, dt):
        return nc.dram_tensor(name, list(shape), dt, kind=skind).ap()

    x_all = din("x_all", [SEQ, D])
    x_own = din("x_own", [NOB * 128, D])
    mem = din("mem", [256, D])
    lnin_g = din("lnin_g", [128, D]); lnin_b = din("lnin_b", [128, D])
    w_in = din("w_in", [D, 4616])
    bfor = din("bfor", [128, 8])
    lnv_g = din("lnv_g", [128, 512]); lnv_b = din("lnv_b", [128, 512])
    wspT = din("wspT", [8, 128, 128])
    bspf = din("bspf", [128, 512])
    w_ba = din("w_ba", [512, D]); w_bb = din("w_bb", [512, D]); w_out = din("w_out", [D, D])
    ln1_g = din("ln1_g", [128, D]); ln1_b = din("ln1_b", [128, D])
    lnm_g = din("lnm_g", [128, D]); lnm_b = din("lnm_b", [128, D])
    w_mq = din("w_mq", [D, D]); w_mkv = din("w_mkv", [D, 2 * D]); w_mo = din("w_mo", [D, D])
    ln2_g = din("ln2_g", [128, D]); ln2_b = din("ln2_b", [128, D])
    w_r = din("w_r", [D, NE]); b_r = din("b_r", [128, NE])
    w_gu = din("w_gu", [NE, D, 2 * D]); b_gu = din("b_gu", [NE, 128, 16])
    w_d = din("w_d", [NE, D, D]); b_d = din("b_d", [NE, D])
    ln3_g = din("ln3_g", [128, D]); ln3_b = din("ln3_b", [128, D])
    ident_f_d = din("ident_f", [128, 128]); ident_b_d = din("ident_b", [128, 128], BF16)
    ut_d = din("ut", [128, 128]); ones_f_d = din("ones_f", [128, 128]); ones_b_d = din("ones_b", [128, 128], BF16)
    cmaskT_d = din("cmaskT", [128, 128])
    maskE_d = din("maskE", [128, 128], BF16); maskO_d = din("maskO", [128, 128], BF16)
    pE_d = din("pE", [128, 1]); pO_d = din("pO", [128, 1]); ecap_d = din("ecap", [128, NE]); trash_d = din("trash", [128, NE])

    out_d = nc.dram_tensor("out", [NOB * 128, D], F32, kind="ExternalOutput").ap()

    Kt_d = dscr("Kt_d", [8, 64, SEQ], BF16)
    V_d = dscr("V_d", [8, 128, NB, 65], BF16)
    Qt_d = dscr("Qt_d", [8, 64, NOB * 128], BF16)
    Ca_d = dscr("Ca_d", [8, NOB * 128], BF16)
    yaT_d = dscr("yaT_d", [NG, 128, 4, 512], BF16)
    ybT_d = dscr("ybT_d", [NG, 128, 4, 512], BF16)
    sg_d = dscr("sg_d", [NG, 128, 16, 512], BF16)
    xn_d = dscr("xn_d", [NOB * 128, D], F32)
    x2_d = dscr("x2_d", [NOB * 128, D], F32)
    xg_d = dscr("xg_d", [NSLOT + 128, D], BF16)
    ye_d = dscr("ye_d", [NSLOT + 128, D], BF16)
    cnt_d = dscr("cnt_d", [128, NE], F32) if debug else None

    es = ExitStack()
    with es:
        esems = {e: es.enter_context(nc.semaphore("sem_" + e)) for e in ("pe", "act", "dve", "pool")}
        dsems = [es.enter_context(nc.semaphore("dsem%d" % i)) for i in range(40)]
        P = Prog(nc, esems, dsems)

        uid = [0]

        def sb(name, shape, dt, stack=es):
            uid[0] += 1
            return stack.enter_context(nc.sbuf_tensor("s%d_%s" % (uid[0], name), list(shape), dt))

        psf = [es.enter_context(nc.psum_tensor("psf%d" % i, [128, 512], F32)) for i in range(6)]
        psb = [es.enter_context(nc.psum_tensor("psb%d" % i, [128, 1024], BF16)) for i in range(2)]

        ident_f = sb("ident_f", [128, 128], F32); ident_b = sb("ident_b", [128, 128], BF16)
        ut = sb("ut", [128, 128], F32); ones_f = sb("ones_f", [128, 128], F32); ones_b = sb("ones_b", [128, 128], BF16)
        maskE = sb("maskE", [128, 128], BF16); maskO = sb("maskO", [128, 128], BF16)
        pE = sb("pE", [128, 1], F32); pO = sb("pO", [128, 1], F32)
        negc_all = sb("negc_all", [128, NB, 8], F32)
        negc_own = sb("negc_own", [128, NOB, 8], F32)
        gw4 = sb("gw4", [128, NOB, 4], F32)
        dest4 = sb("dest4", [128, NOB, 4], I32)
        kmT = sb("kmT", [128, 8, 256], BF16)
        vmem = sb("vmem", [128, 2, 1024], BF16)
        lnt = [{"st": sb("ln_st", [128, 12], F32), "mv": sb("ln_mv", [128, 2], F32), "rstd": sb("ln_rstd", [128, 1], F32),
                "lnv": sb("ln_lnv", [128, 1], F32), "nmr": sb("ln_nmr", [128, 1], F32), "eps": sb("ln_eps", [128, 1], F32)}
               for _ in range(3)]
        P.memset("dve", lnt[0]["eps"][:], EPS, ["lneps"])

        ph12 = ExitStack()
        wu = sb("wu", [128, 8, 512], BF16, ph12); wvg = sb("wvg", [128, 8, 512], BF16, ph12)
        wq = sb("wq", [128, 8, 512], BF16, ph12); wg = sb("wg", [128, 8, 2048], BF16, ph12)
        with ExitStack() as ph:
            def psb_(name, shape, dt):
                return sb(name, shape, dt, ph)
            for i, (t, d_) in enumerate([(ident_f, ident_f_d), (ident_b, ident_b_d), (ut, ut_d), (ones_f, ones_f_d),
                                         (ones_b, ones_b_d), (maskE, maskE_d), (maskO, maskO_d), (pE, pE_d), (pO, pO_d)]):
                P.dma("sp", t[:], d_, "const", [], ["const"])
            lng = psb_("lng", [128, D], F32); lnb = psb_("lnb", [128, D], F32)
            P.dma("sp", lng[:], lnin_g, "const", [], ["lnp"])
            P.dma("sp", lnb[:], lnin_b, "const", [], ["lnp"])
            bfo = psb_("bfo", [128, 8], F32)
            P.dma("sp", bfo[:], bfor, "const", [], ["lnp"])
            wk = psb_("wk", [128, 8, 512], BF16); wv = psb_("wv", [128, 8, 512], BF16); wf = psb_("wf", [128, 8, 8], BF16)
            P.dma("pool", wk[:], w_in[:, 1536:2048].rearrange("(kc p) c -> p kc c", p=128), "wld0", [], ["wk"])
            P.dma("pool", wv[:], w_in[:, 2048:2560].rearrange("(kc p) c -> p kc c", p=128), "wld1", [], ["wv"])
            P.dma("pool", wf[:], w_in[:, 2560:2568].rearrange("(kc p) c -> p kc c", p=128), "wld2", [], ["wf"])
            P.dma("pool", wu[:], w_in[:, 0:512].rearrange("(kc p) c -> p kc c", p=128), "wldu", [], ["wu"])
            P.dma("pool", wvg[:], w_in[:, 512:1024].rearrange("(kc p) c -> p kc c", p=128), "wldvg", [], ["wvg"])
            P.dma("pool", wq[:], w_in[:, 1024:1536].rearrange("(kc p) c -> p kc c", p=128), "wldq", [], ["wq"])
            for i in range(4):
                P.dma("pool", wg[:, :, i * 512:(i + 1) * 512],
                      w_in[:, 2568 + i * 512:2568 + (i + 1) * 512].rearrange("(kc p) c -> p kc c", p=128),
                      "wldg", [], [("wg", i)])
            xt = [psb_("xt%d" % i, [128, D], F32) for i in range(4)]
            xnb = [psb_("xnb%d" % i, [128, D], BF16) for i in range(4)]
            xnT = [psb_("xnT%d" % i, [128, 8, 512], BF16) for i in range(2)]
            ktst = [psb_("ktst%d" % i, [64, 8, 512], BF16) for i in range(2)]
            vst = [psb_("vst%d" % i, [128, 8, 4, 65], BF16) for i in range(2)]
            Cb = psb_("Cb", [128, 8], F32)
            fl = psb_("fl", [128, 8], F32); fe = psb_("fe", [128, 8], F32); fsp = psb_("fsp", [128, 8], F32)
            P.memset("dve", Cb[:], 0.0, ["Cb"])
            for i in range(2):
                P.memset("pool", vst[i][:, :, :, 64:65], 1.0, [("vst", i)])

            nblk_kv = NB if stop_after >= 1 else 8
            xcount = 0

            def load_x(src, blk, cnt):
                s = cnt % 4
                P.dma("sp", xt[s][:], src[blk * 128:(blk + 1) * 128, :], "xt%d" % s, [], [("xt", s)])

            def ln_and_transpose(cnt, bi, gslot, resid=None):
                s = cnt % 4
                nbs = cnt % 4
                P.layernorm(xt[s][:], xt[s][:], lng[:], lnb[:], D, lnt, [("xt", s)], [("xt", s)], ["lnp"], "l")
                if resid is not None:
                    resid(s)
                P.copy("act", xnb[nbs][:], xt[s][:], [("xt", s)], [("xnb", nbs)])
                pb = psb[cnt % 2]
                for kc in range(8):
                    P.tr(pb[:, kc * 128:(kc + 1) * 128], xnb[nbs][:, kc * 128:(kc + 1) * 128], ident_b[:],
                         [("xnb", nbs), "const"], [("psb", cnt % 2)])
                P.copy("dve", xnT[gslot][:, :, bi * 128:(bi + 1) * 128],
                       pb[:].rearrange("p (k t) -> p k t", k=8), [("psb", cnt % 2)], [("xnT", gslot, bi)])

            ngrp = nblk_kv // 4
            load_x(x_all, 0, 0)
            load_x(x_all, 1, 1)
            load_x(x_all, 2, 2)
            cnt = 0
            for g in range(ngrp):
                gs = g % 2
                for bi in range(4):
                    blk = g * 4 + bi
                    if blk + 3 < nblk_kv:
                        load_x(x_all, blk + 3, cnt + 3)
                    ln_and_transpose(cnt, bi, gs)
                    cnt += 1
                xk = [("xnT", gs, bi) for bi in range(4)]
                for h in range(8):
                    pk = psf[h % 2]
                    for kc in range(8):
                        P.mm(pk[0:64, :], wk[:, kc, h * 64:(h + 1) * 64], xnT[gs][:, kc, :], kc == 0, kc == 7,
                             xk + ["wk"], [("psf", h % 2)])
                    P.copy("act" if h % 2 == 0 else "dve", ktst[gs][:, h, :], pk[0:64, :], [("psf", h % 2)], [("ktst", gs)])
                P.dma("sp", Kt_d[:, :, g * 512:(g + 1) * 512].rearrange("h d t -> d h t"), ktst[gs][:], "kst%d" % gs,
                      [("ktst", gs)], [("Kt_d", g)])
                for bi in range(4):
                    pv = psf[2 + bi % 2]
                    for kc in range(8):
                        P.mm(pv[:], xnT[gs][:, kc, bi * 128:(bi + 1) * 128], wv[:, kc, :], kc == 0, kc == 7,
                             [("xnT", gs, bi), "wv"], [("psf", 2 + bi % 2)])
                    P.copy("act" if bi % 2 == 0 else "dve", vst[gs][:, :, bi, 0:64],
                           pv[:].rearrange("p (h d) -> p h d", h=8), [("psf", 2 + bi % 2)], [("vst", gs)])
                P.dma("sp", V_d[:, :, g * 4:(g + 1) * 4, :].rearrange("h p b c -> p h b c"), vst[gs][:], "vst%d" % gs,
                      [("vst", gs)], [("V_d", g)])
                for bi in range(4):
                    blk = g * 4 + bi
                    pf = psf[4]
                    for kc in range(8):
                        P.mm(pf[:, 0:8], xnT[gs][:, kc, bi * 128:(bi + 1) * 128], wf[:, kc, :], kc == 0, kc == 7,
                             [("xnT", gs, bi), "wf"], [("psf", 4)])
                    P.tt("dve", fl[:], pf[:, 0:8], bfo[:], ALU.add, [("psf", 4), "lnp"], ["fl"])
                    P.actf(fe[:], fl[:], AF.Exp, ["fl"], ["fe"], scale=-1.0)
                    P.actf(fsp[:], fe[:], AF.Ln, ["fe"], ["fsp"], bias=1.0)
                    pc = psf[5]
                    P.mm(pc[:, 0:8], ut[:], fsp[:], True, True, ["fsp", "const"], [("psf", 5)])
                    P.mm(pc[:, 8:16], ones_f[:], fsp[:], True, True, ["fsp", "const"], [("psf", 5)])
                    P.tt("dve", negc_all[:, blk, :], pc[:, 0:8], Cb[:], ALU.add, [("psf", 5), "Cb"], [("negc", blk)])
                    P.tt("dve", Cb[:], pc[:, 8:16], Cb[:], ALU.add, [("psf", 5), "Cb"], ["Cb"])
            nv = negc_all[:].rearrange("p (m two) h -> p m two h", two=2)
            allneg = [("negc", b) for b in range(nblk_kv)]
            P.ts("dve", negc_own[:], nv[:, :, 0, :], pE[:, 0:1], None, ALU.mult, None, allneg + ["const"], ["negc_own"])
            P.stt("dve", negc_own[:], nv[:, :, 1, :], pO[:, 0:1], negc_own[:], ALU.mult, ALU.add,
                  allneg + ["const", "negc_own"], ["negc_own"])
            cst = psb_("cst", [8, NOB * 128], BF16)
            for m4 in range(NOB // 4):
                pt_ = psf[m4 % 2]
                for j in range(4):
                    m = m4 * 4 + j
                    P.tr(pt_[0:8, j * 128:(j + 1) * 128], negc_own[:, m, :], ident_f[:], ["negc_own", "const"],
                         [("psf", m4 % 2)])
                P.actf(cst[:, m4 * 512:(m4 + 1) * 512], pt_[0:8, :], AF.Copy, [("psf", m4 % 2)], ["cst"], scale=-8.0)
            P.dma("sp", Ca_d, cst[:], "cst", ["cst"], ["Ca_d"])
            P.emit()

        if stop_after < 2:
            return nc, ins

        with ExitStack() as ph:
            def psb_(name, shape, dt):
                return sb(name, shape, dt, ph)
            lng = psb_("lng", [128, D], F32); lnb = psb_("lnb", [128, D], F32)
            P.dma("sp", lng[:], lnin_g, "const", [], ["lnp"])
            P.dma("sp", lnb[:], lnin_b, "const", [], ["lnp"])
            lvg = psb_("lvg", [128, 512], F32); lvb = psb_("lvb", [128, 512], F32); bsp = psb_("bsp", [128, 512], F32)
            P.dma("sp", lvg[:], lnv_g, "const", [], ["lnp"])
            P.dma("sp", lvb[:], lnv_b, "const", [], ["lnp"])
            P.dma("sp", bsp[:], bspf, "const", [], ["lnp"])
            cmT = psb_("cmT", [128, 128], F32)
            P.dma("sp", cmT[:], cmaskT_d, "const", [], ["lnp"])
            wsf = psb_("wsf", [128, 8, 128], F32); wsb = psb_("wsb", [128, 8, 128], BF16)
            P.dma("sp", wsf[:], wspT.rearrange("g s t -> s g t"), "const", [], ["wsf"])
            for gi in range(8):
                P.tt("dve", wsb[:, gi, :], wsf[:, gi, :], cmT[:], ALU.mult, ["wsf", "lnp"], ["wsb"])
            xt = [psb_("xt%d" % i, [128, D], F32) for i in range(4)]
            xnb = [psb_("xnb%d" % i, [128, D], BF16) for i in range(4)]
            xnT = [psb_("xnT%d" % i, [128, 8, 512], BF16) for i in range(2)]
            qtst = [psb_("qtst%d" % i, [64, 8, 512], BF16) for i in range(2)]
            sgst = [psb_("sgst%d" % i, [128, 16, 512], BF16) for i in range(2)]
            yaTs = [psb_("yaTs%d" % i, [128, 4, 512], BF16) for i in range(2)]
            u_sb = psb_("u_sb", [128, 512], F32); vgl = psb_("vgl", [128, 512], F32)
            vln = psb_("vln", [128, 512], BF16); tmpa = psb_("tmpa", [128, 512], F32); ya = psb_("ya", [128, 512], BF16)

            ng2 = NG if stop_after >= 2 else 1
            nblk = ng2 * 4
            load_x(x_own, 0, 0)
            load_x(x_own, 1, 1)
            load_x(x_own, 2, 2)
            cnt = 0
            for g in range(ng2):
                gs = g % 2
                for bi in range(4):
                    blk = g * 4 + bi
                    if blk + 3 < nblk:
                        load_x(x_own, blk + 3, cnt + 3)

                    def resid(s, blk=blk):
                        P.dma("sp", xn_d[blk * 128:(blk + 1) * 128, :], xt[s][:], "xnst%d" % s, [("xt", s)], [("xn_d", blk)])
                    ln_and_transpose(cnt, bi, gs, resid)
                    cnt += 1
                    xkb = [("xnT", gs, bi)]
                    pu = psf[0]
                    for kc in range(8):
                        P.mm(pu[:], xnT[gs][:, kc, bi * 128:(bi + 1) * 128], wu[:, kc, :], kc == 0, kc == 7,
                             xkb + ["wu"], [("psf", 0)])
                    P.actf(u_sb[:], pu[:], AF.Gelu, [("psf", 0)], ["u_sb"])
                    pv = psf[1]
                    for kc in range(8):
                        P.mm(pv[:], xnT[gs][:, kc, bi * 128:(bi + 1) * 128], wvg[:, kc, :], kc == 0, kc == 7,
                             xkb + ["wvg"], [("psf", 1)])
                    P.actf(vgl[:], pv[:], AF.Gelu, [("psf", 1)], ["vgl"])
                    P.layernorm(vgl[:], vgl[:], lvg[:], lvb[:], 512, lnt, ["vgl"], ["vgl"], ["lnp"], "l")
                    P.copy("pool", vln[:], vgl[:], ["vgl"], ["vln"])
                    pm = psf[2]
                    for gi in range(8):
                        P.mm(pm[:, gi * 64:(gi + 1) * 64], wsb[:, gi, :], vln[:, gi * 64:(gi + 1) * 64], True, True,
                             ["vln", "wsb"], [("psf", 2)])
                    P.tt("dve", tmpa[:], pm[:], bsp[:], ALU.add, [("psf", 2), "lnp"], ["tmpa"])
                    P.tt("pool", ya[:], tmpa[:], u_sb[:], ALU.mult, ["tmpa", "u_sb"], ["ya"])
                    pb = psb[cnt % 2]
                    for kc in range(4):
                        P.tr(pb[:, kc * 128:(kc + 1) * 128], ya[:, kc * 128:(kc + 1) * 128], ident_b[:],
                             ["ya", "const"], [("psb", cnt % 2)])
                    P.copy("dve", yaTs[gs][:, :, bi * 128:(bi + 1) * 128],
                           pb[:, 0:512].rearrange("p (k t) -> p k t", k=4), [("psb", cnt % 2)], [("yaTs", gs)])
                xk = [("xnT", gs, bi) for bi in range(4)]
                P.dma("sp", yaT_d[g], yaTs[gs][:], "yaTs%d" % gs, [("yaTs", gs)], [("yaT_d", g)])
                for h in range(8):
                    pk = psf[3 + h % 2]
                    for kc in range(8):
                        P.mm(pk[0:64, :], wq[:, kc, h * 64:(h + 1) * 64], xnT[gs][:, kc, :], kc == 0, kc == 7,
                             xk + ["wq"], [("psf", 3 + h % 2)])
                    P.copy("dve" if h % 2 == 0 else "act", qtst[gs][:, h, :], pk[0:64, :], [("psf", 3 + h % 2)],
                           [("qtst", gs)])
                P.dma("sp", Qt_d[:, :, g * 512:(g + 1) * 512].rearrange("h d t -> d h t"), qtst[gs][:], "qst%d" % gs,
                      [("qtst", gs)], [("Qt_d", g)])
                for cc in range(16):
                    pg = psf[3 + cc % 3]
                    for kc in range(8):
                        P.mm(pg[:], wg[:, kc, cc * 128:(cc + 1) * 128], xnT[gs][:, kc, :], kc == 0, kc == 7,
                             xk + ["wg"], [("psf", 3 + cc % 3)])
                    P.actf(sgst[gs][:, cc, :], pg[:], AF.Sigmoid, [("psf", 3 + cc % 3)], [("sgst", gs)])
                P.dma("sp", sg_d[g], sgst[gs][:], "sgst%d" % gs, [("sgst", gs)], [("sg_d", g)])
            P.emit()
        ph12.close()
        if stop_after < 3:
            return nc, ins

        ph45 = ExitStack()
        wba = sb("wba", [128, 4, D], BF16, ph45); wbb = sb("wbb", [128, 4, D], BF16, ph45)
        wout = sb("wout", [128, 8, D], BF16, ph45); wmq = sb("wmq", [128, 8, D], BF16, ph45); wmo = sb("wmo", [128, 8, D], BF16, ph45)
        with ExitStack() as ph:
            def psb_(name, shape, dt):
                return sb(name, shape, dt, ph)
            for i in range(2):
                P.dma("pool", wba[:, :, i * 512:(i + 1) * 512], w_ba[:, i * 512:(i + 1) * 512].rearrange("(kc p) c -> p kc c", p=128), "w0", [], [("wba", i)])
                P.dma("pool", wbb[:, :, i * 512:(i + 1) * 512], w_bb[:, i * 512:(i + 1) * 512].rearrange("(kc p) c -> p kc c", p=128), "w1", [], [("wbb", i)])
                P.dma("pool", wout[:, :, i * 512:(i + 1) * 512], w_out[:, i * 512:(i + 1) * 512].rearrange("(kc p) c -> p kc c", p=128), "w2", [], [("wout", i)])
                P.dma("pool", wmq[:, :, i * 512:(i + 1) * 512], w_mq[:, i * 512:(i + 1) * 512].rearrange("(kc p) c -> p kc c", p=128), "w3", [], [("wmq", i)])
                P.dma("pool", wmo[:, :, i * 512:(i + 1) * 512], w_mo[:, i * 512:(i + 1) * 512].rearrange("(kc p) c -> p kc c", p=128), "w4", [], [("wmo", i)])
            P.latadd = 0.2
            ka = [psb_("ka%d" % i, [65, SEQ], BF16) for i in range(2)]
            va = [psb_("va%d" % i, [128, NB, 65], BF16) for i in range(2)]
            qa = [psb_("qa%d" % i, [65, 512], BF16) for i in range(2)]
            pts = [psb_("pt%d" % i, [128, 512], BF16) for i in range(4)]
            yb = psb_("yb", [128, 4, 512], BF16)
            ybTs = [psb_("ybTs%d" % i, [128, 4, 512], BF16) for i in range(2)]
            rs = psb_("rs", [128, 4], F32)
            for i in range(2):
                P.memset("pool", ka[i][64:65, :], 1.0, [("karow", i)])
            ng4 = NG if stop_after >= 4 else 1
            nit = ng4 * 8

            def att_load(it):
                g, h = it // 8, it % 8
                s = it % 2
                nkb = 8 * g + 8
                P.dma("sp", ka[s][0:64, 0:nkb * 128], Kt_d[h, :, 0:nkb * 128], "ka%d" % s, [], [("ka", s)])
                P.dma("sp", va[s][:, 0:nkb, :], V_d[h, :, 0:nkb, :], "va%d" % s, [], [("va", s)])
                P.dma("sp", qa[s][0:64, :], Qt_d[h, :, g * 512:(g + 1) * 512], "qa%d" % s, [], [("qa", s)])
                P.dma("sp", qa[s][64:65, :], Ca_d[h:h + 1, g * 512:(g + 1) * 512], "qa%d" % s, [], [("qa", s)])

            att_load(0)
            pcount = 0
            for it in range(nit):
                g, h = it // 8, it % 8
                s = it % 2
                gs = g % 2
                nkb = 8 * g + 8
                if it + 1 < nit:
                    att_load(it + 1)
                ob = 3 + it % 2
                O = psf[ob]
                O3 = O[:, 0:260].rearrange("p (q c) -> p q c", c=65)

                def qmin(j):
                    return max(0, (j - 8 * g - 1 + 1) // 2)

                def s_mm(j):
                    a = qmin(j) * 128
                    S = psf[j % 3]
                    msk = []
                    for qi in range(qmin(j), 4):
                        if j == 8 * g + 2 * qi:
                            msk.append((qi, maskE))
                        elif j == 8 * g + 2 * qi + 1:
                            msk.append((qi, maskO))
                    P.mm(S[:, a:512], ka[s][0:65, j * 128:(j + 1) * 128], qa[s][0:65, a:512], True, len(msk) == 0,
                         [("ka", s), ("karow", s), ("qa", s)], [("psf", j % 3)])
                    for n, (qi, mk) in enumerate(msk):
                        P.mm(S[:, qi * 128:(qi + 1) * 128], ident_b[:], mk[:], False, n == len(msk) - 1,
                             [], [("psf", j % 3)])

                s_mm(0)
                if nkb > 1:
                    s_mm(1)
                for j in range(nkb):
                    a = qmin(j) * 128
                    pi = pcount % 4
                    pcount += 1
                    P.actf(pts[pi][:, a:512], psf[j % 3][:, a:512], AF.Exp, [("psf", j % 3)], [("pt", pi)],
                           bias=negc_all[:, j, h:h + 1], scale=0.125)
                    if j + 2 < nkb:
                        s_mm(j + 2)
                    for qi in range(qmin(j), 4):
                        P.mm(O3[:, qi, :], pts[pi][:, qi * 128:(qi + 1) * 128], va[s][:, j, :],
                             (j == 0 and qi == 0), j == 8 * g + 2 * qi + 1, [("pt", pi), ("va", s)], [("psf", ob)])
                P.add("dve", lambda e, O3=O3: e.reciprocal(out=rs[:, 0:4], in_=O3[:, :, 64]), [("psf", ob)], ["rs"])
                for qi in range(4):
                    P.ts("dve", yb[:, qi, h * 64:(h + 1) * 64], O3[:, qi, 0:64], rs[:, qi:qi + 1], 1.0, ALU.mult, ALU.mult,
                         [("psf", ob), "rs"], [("yb", qi)])
                if h == 7:
                    for qi in range(4):
                        pb = psb[qi % 2]
                        for kc in range(4):
                            P.tr(pb[:, kc * 128:(kc + 1) * 128], yb[:, qi, kc * 128:(kc + 1) * 128], ident_b[:],
                                 [("yb", qi)], [("psb", qi % 2)])
                        P.copy("dve", ybTs[gs][:, :, qi * 128:(qi + 1) * 128],
                               pb[:, 0:512].rearrange("p (k t) -> p k t", k=4), [("psb", qi % 2)], [("ybTs", gs)])
                    P.dma("sp", ybT_d[g], ybTs[gs][:], "ybTs%d" % gs, [("ybTs", gs)], [("ybT_d", g)])
            P.emit()
        if stop_after < 5:
            return nc, ins

        P.latadd = 0.2
        with ExitStack() as ph:
            def psb_(name, shape, dt):
                return sb(name, shape, dt, ph)
            lmg = psb_("lmg", [128, D], F32); lmb = psb_("lmb", [128, D], F32)
            P.dma("sp", lmg[:], lnm_g, "c1", [], ["lnp"])
            P.dma("sp", lmb[:], lnm_b, "c2", [], ["lnp2"])
            wmkv = psb_("wmkv", [128, 8, 2048], BF16)
            for i in range(4):
                P.dma("pool", wmkv[:, :, i * 512:(i + 1) * 512],
                      w_mkv[:, i * 512:(i + 1) * 512].rearrange("(kc p) c -> p kc c", p=128), "w%d" % i, [], [("wmkv", i)])
            wmk = [("wmkv", i) for i in range(4)]
            mt = [psb_("mt%d" % i, [128, D], F32) for i in range(2)]
            mtb = [psb_("mtb%d" % i, [128, D], BF16) for i in range(2)]
            memT = psb_("memT", [128, 8, 256], BF16)
            for i in range(2):
                P.dma("sp", mt[i][:], mem[i * 128:(i + 1) * 128, :], "mt%d" % i, [], [("mt", i)])
                P.layernorm(mt[i][:], mt[i][:], lmg[:], lmb[:], D, lnt, [("mt", i)], [("mt", i)], ["lnp", "lnp2"], "l")
                P.copy("act", mtb[i][:], mt[i][:], [("mt", i)], [("mtb", i)])
                pb = psb[i]
                for kc in range(8):
                    P.tr(pb[:, kc * 128:(kc + 1) * 128], mtb[i][:, kc * 128:(kc + 1) * 128], ident_b[:], [("mtb", i)], [("psb", i)])
                P.copy("dve", memT[:, :, i * 128:(i + 1) * 128], pb[:].rearrange("p (k t) -> p k t", k=8), [("psb", i)], ["memT"])
            for cc in range(8):
                pk = psf[cc % 2]
                for kc in range(8):
                    P.mm(pk[:, 0:256], wmkv[:, kc, cc * 128:(cc + 1) * 128], memT[:, kc, :], kc == 0, kc == 7,
                         wmk + ["memT"], [("psf", cc % 2)])
                P.copy("act" if cc % 2 == 0 else "dve", kmT[:, cc, :], pk[:, 0:256], [("psf", cc % 2)], ["kmT"])
            for mc in range(2):
                for half in range(2):
                    pv = psf[2 + half]
                    for kc in range(8):
                        P.mm(pv[:], memT[:, kc, mc * 128:(mc + 1) * 128], wmkv[:, kc, 1024 + half * 512:1024 + (half + 1) * 512],
                             kc == 0, kc == 7, wmk + ["memT"], [("psf", 2 + half)])
                    P.copy("act" if half == 0 else "dve", vmem[:, mc, half * 512:(half + 1) * 512], pv[:], [("psf", 2 + half)], ["vmem"])
            P.emit()

        with ExitStack() as ph:
            def psb_(name, shape, dt):
                return sb(name, shape, dt, ph)
            l1g = psb_("l1g", [128, D], F32); l1b = psb_("l1b", [128, D], F32)
            l2g = psb_("l2g", [128, D], F32); l2b = psb_("l2b", [128, D], F32)
            for i, (t, d_) in enumerate([(l1g, ln1_g), (l1b, ln1_b), (l2g, ln2_g), (l2b, ln2_b)]):
                P.dma("sp", t[:], d_, "c%d" % i, [], ["lnp%d" % i])
            lnk = ["lnp%d" % i for i in range(4)]
            wr = psb_("wr", [128, 8, NE], F32); br = psb_("br", [128, NE], F32); ecap = psb_("ecap", [128, NE], F32)
            P.dma("sp", wr[:], w_r.rearrange("(kc p) c -> p kc c", p=128), "c4", [], ["wr"])
            P.dma("sp", br[:], b_r, "c5", [], ["br"])
            P.dma("sp", ecap[:], ecap_d, "c6", [], ["ecap"])
            trash = psb_("trash", [128, NE], F32)
            P.dma("sp", trash[:], trash_d, "c6", [], ["ecap"])
            wk2 = lambda n: [(n, 0), (n, 1)]
            yaT_gs = [psb_("yaT_g%d" % i, [128, 4, 512], BF16) for i in range(2)]
            ybT_gs = [psb_("ybT_g%d" % i, [128, 4, 512], BF16) for i in range(2)]
            sg_g = psb_("sg_g", [128, 16, 512], BF16)
            xrs = [psb_("xr%d" % i, [128, 4, D], F32) for i in range(2)]
            mT = psb_("mT", [128, 8, 512], BF16)
            t1 = [psb_("t1_%d" % i, [128, 512], F32) for i in range(2)]
            t2 = [psb_("t2_%d" % i, [128, 512], F32) for i in range(2)]
            xb16 = [psb_("xb16_%d" % i, [128, D], BF16) for i in range(2)]
            x1T = psb_("x1T", [128, 8, 512], BF16); qmT = mT
            pm = [psb_("pm%d" % i, [128, 512], BF16) for i in range(2)]
            oT = x1T; rinv = psb_("rinv", [128, 512], F32)
            x2b = [psb_("x2b%d" % i, [128, D], BF16) for i in range(2)]
            x2Tf = psb_("x2Tf", [128, 8, 128], F32)
            lg = psb_("lg", [128, NE], F32); top8 = psb_("top8", [128, 8], F32); negm = psb_("negm", [128, 1], F32)
            ex = psb_("ex", [128, NE], F32); msk = psb_("msk", [128, NE], F32); em = psb_("em", [128, NE], F32)
            den = psb_("den", [128, 1], F32); rden = psb_("rden", [128, 1], F32); gwd = psb_("gwd", [128, NE], F32)
            dst = psb_("dst", [128, NE], F32); dst2 = psb_("dst2", [128, NE], F32); pen = psb_("pen", [128, NE], F32)
            base = psb_("base", [128, NE], F32); oh = psb_("oh", [128, NE], F32); tm32 = psb_("tm32", [128, NE], F32)
            d4f = psb_("d4f", [128, 4], F32)
            sidx = [psb_("sidx%d" % i, [128, 1], I32) for i in range(8)]
            P.memset("dve", base[:], 0.0, ["base"])
            if os.environ.get("KVERBOSE") == "1":
                print("phase5 sbuf remaining", nc.sbuf_bytes_remaining)
            ng5 = NG if stop_after >= 5 else 1
            bcount = 0
            for g in range(ng5):
                g2 = g % 2
                yaT_g = yaT_gs[g2]; ybT_g = ybT_gs[g2]; xr = xrs[g2]
                XR = "xr%d" % g2
                P.dma("sp", yaT_g[:], yaT_d[g], "l0_%d" % g2, [], ["yaT_g%d" % g2])
                P.dma("sp", ybT_g[:], ybT_d[g], "l1_%d" % g2, [], ["ybT_g%d" % g2])
                P.dma("sp", sg_g[:], sg_d[g], "l2", [], ["sg_g"])
                xrk = [(XR, bi) for bi in range(4)]
                P.dma("sp", xr[:], xn_d[g * 512:(g + 1) * 512, :].rearrange("(b p) d -> p b d", p=128), "l3_%d" % g2, [], xrk)
                for cc in range(8):
                    A = psf[cc % 2]; B = psf[2 + cc % 2]
                    for kc in range(4):
                        P.mm(A[:], wba[:, kc, cc * 128:(cc + 1) * 128], yaT_g[:, kc, :], kc == 0, kc == 3,
                             wk2("wba") + ["yaT_g%d" % g2], [("psf", cc % 2)])
                    for kc in range(4):
                        P.mm(B[:], wbb[:, kc, cc * 128:(cc + 1) * 128], ybT_g[:, kc, :], kc == 0, kc == 3,
                             wk2("wbb") + ["ybT_g%d" % g2], [("psf", 2 + cc % 2)])
                    P.tt("dve", t1[cc % 2][:], A[:], sg_g[:, cc, :], ALU.mult, [("psf", cc % 2), "sg_g"], [("t1", cc % 2)])
                    P.tt("dve", t2[cc % 2][:], B[:], sg_g[:, 8 + cc, :], ALU.mult, [("psf", 2 + cc % 2), "sg_g"], [("t2", cc % 2)])
                    P.tt("pool", mT[:, cc, :], t1[cc % 2][:], t2[cc % 2][:], ALU.add, [("t1", cc % 2), ("t2", cc % 2)], [("mT", cc)])
                mTk = [("mT", cc) for cc in range(8)]

                def proj(srcT, srck, wt, wname, bi):
                    for half in range(2):
                        pso = psf[4 + half]
                        for kc in range(8):
                            P.mm(pso[:], srcT[:, kc, bi * 128:(bi + 1) * 128], wt[:, kc, half * 512:(half + 1) * 512],
                                 kc == 0, kc == 7, srck + wk2(wname), [("psf", 4 + half)])
                        P.stt("dve", xr[:, bi, half * 512:(half + 1) * 512], xr[:, bi, half * 512:(half + 1) * 512], ALPHA,
                              pso[:], ALU.mult, ALU.add, [("psf", 4 + half), (XR, bi)], [(XR, bi)])

                def lnf(lg_, lb_, bi):
                    P.layernorm(xr[:, bi, :], xr[:, bi, :], lg_[:], lb_[:], D, lnt, [(XR, bi)], [(XR, bi)], lnk, "l")

                proj(mT, mTk, wout, "wout", 0)
                for bi in range(4):
                    if bi + 1 < 4:
                        proj(mT, mTk, wout, "wout", bi + 1)
                    lnf(l1g, l1b, bi)
                    xs = bcount % 2
                    P.copy("act", xb16[xs][:], xr[:, bi, :], [(XR, bi)], [("xb16", xs)])
                    pb = psb[bcount % 2]
                    for kc in range(8):
                        P.tr(pb[:, kc * 128:(kc + 1) * 128], xb16[xs][:, kc * 128:(kc + 1) * 128], ident_b[:],
                             [("xb16", xs)], [("psb", bcount % 2)])
                    P.copy("dve", x1T[:, :, bi * 128:(bi + 1) * 128], pb[:].rearrange("p (k t) -> p k t", k=8),
                           [("psb", bcount % 2)], [("x1T", bi)])
                    bcount += 1
                x1k = [("x1T", bi) for bi in range(4)]
                for cc in range(8):
                    pq = psf[cc % 2]
                    for kc in range(8):
                        P.mm(pq[:], wmq[:, kc, cc * 128:(cc + 1) * 128], x1T[:, kc, :], kc == 0, kc == 7,
                             x1k + wk2("wmq"), [("psf", cc % 2)])
                    P.copy("act" if cc % 2 == 0 else "dve", qmT[:, cc, :], pq[:], [("psf", cc % 2)], [("mT", cc)])
                for hm in range(4):
                    for mc in range(2):
                        S = psf[2 + mc]
                        for dc in range(2):
                            P.mm(S[:], kmT[:, 2 * hm + dc, mc * 128:(mc + 1) * 128], qmT[:, 2 * hm + dc, :], dc == 0, dc == 1,
                                 [("mT", 2 * hm + dc)], [("psf", 2 + mc)])
                        P.actf(pm[mc][:], S[:], AF.Exp, [("psf", 2 + mc)], [("pm", mc)], scale=1.0 / 16.0)
                    R = psf[4]
                    for mc in range(2):
                        P.mm(R[:], ones_b[:], pm[mc][:], mc == 0, mc == 1, [("pm", mc)], [("psf", 4)])
                    P.add("dve", lambda e, R=R: e.reciprocal(out=rinv[:], in_=R[:]), [("psf", 4)], ["rinv"])
                    for dc in range(2):
                        Ops = psf[dc]
                        for mc in range(2):
                            P.mm(Ops[:], vmem[:, mc, hm * 256 + dc * 128:hm * 256 + (dc + 1) * 128], pm[mc][:], mc == 0, mc == 1,
                                 [("pm", mc)], [("psf", dc)])
                        P.tt("dve", oT[:, 2 * hm + dc, :], Ops[:], rinv[:], ALU.mult, [("psf", dc), "rinv"], [("x1T", b_) for b_ in range(4)])
                oTk = [("x1T", b_) for b_ in range(4)]
                proj(oT, oTk, wmo, "wmo", 0)
                for bi in range(4):
                    blk = g * 4 + bi
                    if bi + 1 < 4:
                        proj(oT, oTk, wmo, "wmo", bi + 1)
                    lnf(l2g, l2b, bi)
                    P.dma("sp", x2_d[blk * 128:(blk + 1) * 128, :], xr[:, bi, :], "x2st", [(XR, bi)], [("x2_d", blk)])
                    xs = blk % 2
                    P.copy("act", x2b[xs][:], xr[:, bi, :], [(XR, bi)], [("x2b", xs)])
                    for kc in range(8):
                        pt_ = psf[kc // 4]
                        P.tr(pt_[:, (kc % 4) * 128:(kc % 4 + 1) * 128], xr[:, bi, kc * 128:(kc + 1) * 128], ident_f[:],
                             [(XR, bi)], [("psf", kc // 4)])
                    for hh in range(2):
                        P.copy("dve" if hh == 0 else "act", x2Tf[:, hh * 4:(hh + 1) * 4, :],
                               psf[hh][:].rearrange("p (k t) -> p k t", k=4), [("psf", hh)], [("x2Tf", hh)])
                    pl = psf[2]
                    for kc in range(8):
                        P.mm(pl[:, 0:NE], x2Tf[:, kc, :], wr[:, kc, :], kc == 0, kc == 7, [("x2Tf", 0), ("x2Tf", 1), "wr"], [("psf", 2)])
                    P.tt("dve", lg[:], pl[:, 0:NE], br[:], ALU.add, [("psf", 2), "br"], ["lg"])
                    P.add("dve", lambda e: e.max(out=top8[:], in_=lg[:]), ["lg"], ["top8"])
                    P.ts("dve", negm[:], top8[:, 0:1], -1.0, None, ALU.mult, None, ["top8"], ["negm"])
                    P.actf(ex[:], lg[:], AF.Exp, ["lg", "negm"], ["ex"], bias=negm[:, 0:1])
                    P.ts("dve", msk[:], lg[:], top8[:, 3:4], 1.0, ALU.is_ge, ALU.mult, ["lg", "top8"], ["msk"])
                    P.tt("dve", em[:], ex[:], msk[:], ALU.mult, ["ex", "msk"], ["em"])
                    P.add("dve", lambda e: e.tensor_reduce(out=den[:], in_=em[:], axis=AX.X, op=ALU.add), ["em"], ["den"])
                    P.add("dve", lambda e: e.reciprocal(out=rden[:], in_=den[:]), ["den"], ["rden"])
                    P.ts("dve", gwd[:], em[:], rden[:, 0:1], 1.0, ALU.mult, ALU.mult, ["em", "rden"], ["gwd"])
                    pc = psf[3]
                    P.mm(pc[:, 0:NE], ut[:], msk[:], True, True, ["msk"], [("psf", 3)])
                    P.mm(pc[:, NE:2 * NE], ones_f[:], msk[:], True, True, ["msk"], [("psf", 3)])
                    P.tt("dve", dst[:], pc[:, 0:NE], base[:], ALU.add, [("psf", 3), "base"], ["dst"])
                    P.tt("dve", base[:], pc[:, NE:2 * NE], base[:], ALU.add, [("psf", 3), "base"], ["base"])
                    P.ts("dve", pen[:], dst[:], CAP + 0.5, 1.0, ALU.is_gt, ALU.mult, ["dst"], ["pen"])
                    P.stt("dve", dst2[:], dst[:], -1.0, ecap[:], ALU.add, ALU.add, ["dst", "ecap"], ["dst2"])
                    P.tt("dve", tm32[:], trash[:], dst2[:], ALU.subtract, ["dst2", "ecap"], ["tm32"])
                    P.tt("dve", tm32[:], tm32[:], pen[:], ALU.mult, ["tm32", "pen"], ["tm32"])
                    P.tt("dve", dst2[:], dst2[:], tm32[:], ALU.add, ["dst2", "tm32"], ["dst2"])
                    for k in range(4):
                        P.ts("dve", oh[:], lg[:], top8[:, k:k + 1], 1.0, ALU.is_equal, ALU.mult, ["lg", "top8"], ["oh"])
                        P.tt("dve", tm32[:], oh[:], dst2[:], ALU.mult, ["oh", "dst2"], ["tm32"])
                        P.add("dve", (lambda k: lambda e: e.tensor_reduce(out=d4f[:, k:k + 1], in_=tm32[:], axis=AX.X, op=ALU.add))(k),
                              ["tm32"], ["d4f"])
                        P.tt("dve", tm32[:], oh[:], gwd[:], ALU.mult, ["oh", "gwd", "d4f"], ["tm32"])
                        P.add("dve", (lambda k, blk: lambda e: e.tensor_reduce(out=gw4[:, blk, k:k + 1], in_=tm32[:], axis=AX.X, op=ALU.add))(k, blk),
                              ["tm32"], [("gw4", blk)])
                    P.copy("dve", dest4[:, blk, :], d4f[:], ["d4f"], [("dest4", blk)])
                    for k in range(4):
                        si = (blk % 2) * 4 + k
                        P.copy("dve", sidx[si][:], d4f[:, k:k + 1], ["d4f"], [("sidx", si)])
                        P.add("pool", (lambda si, xs: lambda e: e.indirect_dma_start(
                            out=xg_d, out_offset=bass.IndirectOffsetOnAxis(ap=sidx[si][:, :], axis=0),
                            in_=x2b[xs][:, :], in_offset=None))(si, xs),
                            [("sidx", si), ("x2b", xs)], [("xg_d", blk, k)], dma="scat%d" % xs)
            if debug:
                P.dma("sp", cnt_d, base[:], "cntd", ["base"], ["cnt_d"])
            P.emit()
        ph45.close()
        if stop_after < 6:
            return nc, ins

        NT = CAP // 128
        cgs = []
        c0 = 0
        while c0 < CAP:
            cgs.append((c0, min(512, CAP - c0)))
            c0 += 512
        with ExitStack() as ph:
            def psb_(name, shape, dt):
                return sb(name, shape, dt, ph)
            wgu = [psb_("wgu%d" % i, [128, 8, 2 * D], BF16) for i in range(2)]
            wdn = [psb_("wdn%d" % i, [128, 8, D], BF16) for i in range(2)]
            bgu = [psb_("bgu%d" % i, [128, 16], F32) for i in range(2)]
            bdb = [psb_("bdb%d" % i, [128, D], F32) for i in range(2)]
            xgt = [psb_("xgt%d" % i, [128, D], BF16) for i in range(3)]
            xgT = psb_("xgT", [128, 8, CAP], BF16)
            actT = psb_("actT", [128, 8, CAP], BF16)
            gq = [psb_("gq%d" % i, [128, 512], F32) for i in range(2)]
            sq = [psb_("sq%d" % i, [128, 512], F32) for i in range(2)]
            uq = [psb_("uq%d" % i, [128, 512], F32) for i in range(2)]
            ys = [psb_("ys%d" % i, [128, D], BF16) for i in range(2)]
            zt = psb_("zt", [128, D], BF16)
            P.memset("pool", zt[:], 0.0, ["zt"])
            P.dma("sp", ye_d[NSLOT:NSLOT + 128, :], zt[:], "zt", ["zt"], ["ye_trash"])
            ne6 = NE if stop_after >= 6 else 2

            def exp_load(e):
                s_ = e % 2
                for i in range(4):
                    P.dma("pool", wgu[s_][:, :, i * 512:(i + 1) * 512],
                          w_gu[e][:, i * 512:(i + 1) * 512].rearrange("(kc p) c -> p kc c", p=128), "wgu%d_%d" % (s_, i), [], [("wgu", s_, i)])
                for i in range(2):
                    P.dma("pool", wdn[s_][:, :, i * 512:(i + 1) * 512],
                          w_d[e][:, i * 512:(i + 1) * 512].rearrange("(kc p) c -> p kc c", p=128), "wdn%d_%d" % (s_, i), [], [("wdn", s_, i)])
                P.dma("sp", bdb[s_][:], b_d[e:e + 1, :].to_broadcast([128, D]), "bdb%d" % s_, [], [("bdb", s_)])
                P.dma("sp", bgu[s_][:], b_gu[e], "bgu%d" % s_, [], [("bgu", s_)])

            exp_load(0)
            tcount = 0
            ecount = 0
            ycount = 0
            for e in range(ne6):
                s_ = e % 2
                if e + 1 < ne6:
                    exp_load(e + 1)
                for t in range(NT):
                    xs = tcount % 3
                    P.dma("sp", xgt[xs][:], xg_d[e * CAP + t * 128:e * CAP + (t + 1) * 128, :], "xgt%d" % xs, [], [("xgt", xs)])
                    pb = psb[tcount % 2]
                    for kc in range(8):
                        P.tr(pb[:, kc * 128:(kc + 1) * 128], xgt[xs][:, kc * 128:(kc + 1) * 128], ident_b[:], [("xgt", xs)], [("psb", tcount % 2)])
                    P.copy("dve" if t % 2 == 0 else "act", xgT[:, :, t * 128:(t + 1) * 128], pb[:].rearrange("p (k t) -> p k t", k=8),
                           [("psb", tcount % 2)], [("xgT", t)])
                    tcount += 1
                wguk = [("wgu", s_, i) for i in range(4)]
                wdnk = [("wdn", s_, i) for i in range(2)]
                for fc in range(8):
                    for (c0, ncol) in cgs:
                        b0 = ecount % 2
                        ecount += 1
                        G = psf[b0]; U = psf[2 + b0]
                        xk = [("xgT", t) for t in range(c0 // 128, (c0 + ncol) // 128)]
                        for kc in range(8):
                            P.mm(G[:, 0:ncol], wgu[s_][:, kc, fc * 128:(fc + 1) * 128], xgT[:, kc, c0:c0 + ncol], kc == 0, kc == 7,
                                 xk + wguk, [("psf", b0)])
                        for kc in range(8):
                            P.mm(U[:, 0:ncol], wgu[s_][:, kc, D + fc * 128:D + (fc + 1) * 128], xgT[:, kc, c0:c0 + ncol], kc == 0, kc == 7,
                                 xk + wguk, [("psf", 2 + b0)])
                        P.ts("dve", gq[b0][:, 0:ncol], G[:, 0:ncol], bgu[s_][:, fc:fc + 1], 7.0, ALU.add, ALU.min,
                             [("psf", b0), ("bgu", s_)], [("gq", b0)])
                        P.actf(sq[b0][:, 0:ncol], gq[b0][:, 0:ncol], AF.Sigmoid, [("gq", b0)], [("sq", b0)], scale=1.702)
                        P.ts("dve", uq[b0][:, 0:ncol], U[:, 0:ncol], bgu[s_][:, 8 + fc:9 + fc], 7.0, ALU.add, ALU.min,
                             [("psf", 2 + b0), ("bgu", s_)], [("uq", b0)])
                        P.ts("dve", uq[b0][:, 0:ncol], uq[b0][:, 0:ncol], -7.0, 1.0, ALU.max, ALU.add, [("uq", b0)], [("uq", b0)])
                        P.tt("pool", gq[b0][:, 0:ncol], gq[b0][:, 0:ncol], sq[b0][:, 0:ncol], ALU.mult, [("gq", b0), ("sq", b0)], [("gq", b0)])
                        P.tt("dve", actT[:, fc, c0:c0 + ncol], uq[b0][:, 0:ncol], gq[b0][:, 0:ncol], ALU.mult,
                             [("uq", b0), ("gq", b0)], [("actT", fc)])
                ak = [("actT", fc) for fc in range(8)]
                for t in range(NT):
                    ysl = ycount % 2
                    ycount += 1
                    for half in range(2):
                        Y = psf[4 + half]
                        for fc in range(8):
                            P.mm(Y[:], actT[:, fc, t * 128:(t + 1) * 128], wdn[s_][:, fc, half * 512:(half + 1) * 512], fc == 0, fc == 7,
                                 ak + wdnk, [("psf", 4 + half)])
                        P.tt("dve", ys[ysl][:, half * 512:(half + 1) * 512], Y[:], bdb[s_][:, half * 512:(half + 1) * 512], ALU.add,
                             [("psf", 4 + half), ("bdb", s_)], [("ys", ysl)])
                    P.dma("sp", ye_d[e * CAP + t * 128:e * CAP + (t + 1) * 128, :], ys[ysl][:], "ys%d" % ysl, [("ys", ysl)], [("ye_d", e, t)])
            P.emit()
        if stop_after < 7:
            return nc, ins

        with ExitStack() as ph:
            def psb_(name, shape, dt):
                return sb(name, shape, dt, ph)
            l3g = psb_("l3g", [128, D], F32); l3b = psb_("l3b", [128, D], F32)
            P.dma("sp", l3g[:], ln3_g, "c0", [], ["lnp0"])
            P.dma("sp", l3b[:], ln3_b, "c1", [], ["lnp1"])
            yg = [[psb_("yg%d_%d" % (i, k), [128, D], BF16) for k in range(4)] for i in range(3)]
            gidx = [[psb_("gidx%d_%d" % (i, k), [128, 1], I32) for k in range(4)] for i in range(3)]
            x2t = [psb_("x2t%d" % i, [128, D], F32) for i in range(3)]
            for blk in range(NOB):
                sl = blk % 3
                P.dma("sp", x2t[sl][:], x2_d[blk * 128:(blk + 1) * 128, :], "x2t%d" % sl, [], [("x2t", sl)])
                for k in range(4):
                    P.copy("dve", gidx[sl][k][:], dest4[:, blk, k:k + 1], [], [("gidx", sl, k)])
                    P.add("pool", (lambda sl, k: lambda e: e.indirect_dma_start(
                        out=yg[sl][k][:, :], out_offset=None, in_=ye_d,
                        in_offset=bass.IndirectOffsetOnAxis(ap=gidx[sl][k][:, :], axis=0)))(sl, k),
                        [("gidx", sl, k)], [("yg", sl, k)], dma="yg%d_%d" % (sl, k))
                P.actf(x2t[sl][:], x2t[sl][:], AF.Copy, [("x2t", sl)], [("x2t", sl)], scale=ALPHA)
                for k in range(4):
                    P.stt("dve", x2t[sl][:], yg[sl][k][:], gw4[:, blk, k:k + 1], x2t[sl][:], ALU.mult, ALU.add,
                          [("yg", sl, k), ("x2t", sl)], [("x2t", sl)])
                P.layernorm(x2t[sl][:], x2t[sl][:], l3g[:], l3b[:], D, lnt, [("x2t", sl)], [("x2t", sl)], ["lnp0", "lnp1"], "l")
                P.dma("sp", out_d[blk * 128:(blk + 1) * 128, :], x2t[sl][:], "out%d" % sl, [("x2t", sl)], [("out_d", blk)])
            P.emit()
    return nc, ins


_CACHE = {}


def kernel(**inputs):
    inp = {k: np.asarray(v) for k, v in inputs.items()}
    if "nc" not in _CACHE:
        _CACHE["nc"] = build_program(debug=False)
    nc, ins = _CACHE["nc"]
    rep = lambda v, n=128: np.ascontiguousarray(np.broadcast_to(np.asarray(v, np.float32).reshape(1, -1), (n, np.asarray(v).size)))
    shared = {}
    shared["lnin_g"] = rep(inp["ln_in_g"]); shared["lnin_b"] = rep(inp["ln_in_b"])
    shared["w_in"] = np.ascontiguousarray(inp["w_in"][0]); shared["bfor"] = rep(inp["b_forget"][0])
    shared["lnv_g"] = rep(inp["ln_v_g"][0]); shared["lnv_b"] = rep(inp["ln_v_b"][0])
    shared["wspT"] = np.ascontiguousarray(np.transpose(inp["w_spatial"][0], (0, 2, 1)))
    bs = inp["b_spatial"][0]
    shared["bspf"] = np.ascontiguousarray(np.repeat(bs.T[:, :, None], 64, axis=2).reshape(128, 512)).astype(np.float32)
    shared["w_ba"] = np.ascontiguousarray(inp["w_branch_a"][0]); shared["w_bb"] = np.ascontiguousarray(inp["w_branch_b"][0])
    shared["w_out"] = np.ascontiguousarray(inp["w_out"][0])
    shared["ln1_g"] = rep(inp["ln1_g"][0]); shared["ln1_b"] = rep(inp["ln1_b"][0])
    shared["lnm_g"] = rep(inp["ln_mem_g"][0]); shared["lnm_b"] = rep(inp["ln_mem_b"][0])
    shared["w_mq"] = np.ascontiguousarray(inp["w_mq"][0]); shared["w_mkv"] = np.ascontiguousarray(inp["w_mkv"][0])
    shared["w_mo"] = np.ascontiguousarray(inp["w_mo"][0])
    shared["ln2_g"] = rep(inp["ln2_g"][0]); shared["ln2_b"] = rep(inp["ln2_b"][0])
    shared["w_r"] = np.ascontiguousarray(inp["w_router"][0]); shared["b_r"] = rep(inp["b_router"][0])
    shared["w_gu"] = np.ascontiguousarray(inp["w_gate_up"][0])
    shared["b_gu"] = np.ascontiguousarray(inp["b_gate_up"][0].reshape(NE, 16, 128).transpose(0, 2, 1))
    shared["w_d"] = np.ascontiguousarray(inp["w_down"][0]); shared["b_d"] = np.ascontiguousarray(inp["b_down"][0])
    shared["ln3_g"] = rep(inp["ln3_g"][0]); shared["ln3_b"] = rep(inp["ln3_b"][0])
    consts = [_consts(0), _consts(1)]
    in_maps = []
    for c in range(8):
        b, p = c // 2, c % 2
        x = inp["x"][b]
        d = dict(shared)
        d["x_all"] = np.ascontiguousarray(x)
        d["x_own"] = np.ascontiguousarray(x.reshape(NB, 128, D)[p::2].reshape(NOB * 128, D))
        d["mem"] = np.ascontiguousarray(inp["mem"][b])
        d.update(consts[p])
        in_maps.append({k: v for k, v in d.items() if k in ins})
    res = run_bass_kernel_spmd(nc, in_maps, core_ids=list(range(8)))
    out = np.empty((4, SEQ, D), np.float32)
    for c in range(8):
        b, p = c // 2, c % 2
        out[b].reshape(NB, 128, D)[p::2] = np.asarray(res.results[c]["out"]).reshape(NOB, 128, D)
    return out
```

```python
import os
from contextlib import ExitStack
import numpy as np
import ml_dtypes
import concourse.bass as bass
import concourse.mybir as mybir
from concourse.bass_utils import run_bass_kernel_spmd

F32 = mybir.dt.float32
BF16 = mybir.dt.bfloat16
I32 = mybir.dt.int32
AF = mybir.ActivationFunctionType
ALU = mybir.AluOpType
AX = mybir.AxisListType

D = 1024
SEQ = 8192
NB = 64
NOB = 32
NG = 8
NE = 32
CAP = 896
NSLOT = NE * CAP
ALPHA = 2.0 ** 0.25
EPS = 1e-5
NEG = -30000.0
BIGIDX = 1.0e6

ENGS = ("pe", "act", "dve", "pool", "sp")


class _Op:
    __slots__ = ("eng", "fn", "deps", "odeps", "dma", "inc", "val", "dur", "lat", "idx", "t0", "t1", "nd", "succ")


def _fsz(ap):
    n = 1
    for d in ap.shape[1:]:
        n *= int(d)
    return n


class Prog:
    def __init__(self, nc, esems, dsems):
        self.nc = nc
        self.esems = esems
        self.dsems = dsems
        self.ecnt = {e: 0 for e in ENGS}
        self.dcnt = [0] * len(dsems)
        self.latadd = 0.2
        self.reset_phase()

    def reset_phase(self):
        self.ops = {e: [] for e in ENGS}
        self.allops = []
        self.lastw = {}
        self.rdall = {}
        self.dkey = {}
        self.lastdma = {}

    def add(self, eng, fn, reads=(), writes=(), dma=None, dur=0.3, lat=None):
        op = _Op()
        op.eng = eng
        op.fn = fn
        op.inc = False
        op.val = 0
        op.dma = None
        if dma is not None and lat is None:
            dur, lat = 1.0, 3.5
        op.dur = dur
        op.lat = (dur + self.latadd) if lat is None else lat
        op.odeps = []
        deps = set()
        for k in reads:
            w = self.lastw.get(k)
            if w is not None:
                deps.add(w)
        for k in writes:
            w = self.lastw.get(k)
            if w is not None:
                deps.add(w)
            for r in self.rdall.get(k, ()):
                deps.add(r)
        op.deps = deps
        if dma is not None:
            if dma not in self.dkey:
                self.dkey[dma] = len(self.dkey)
                assert len(self.dkey) <= len(self.dsems), "too many dma keys"
            si = self.dkey[dma]
            self.dcnt[si] += 16
            op.dma = si
            op.val = self.dcnt[si]
            prev = self.lastdma.get(si)
            if prev is not None:
                assert prev.eng == eng, "dma key shared between engines"
                op.odeps.append(prev)
            self.lastdma[si] = op
        for k in reads:
            self.rdall.setdefault(k, []).append(op)
        for k in writes:
            self.lastw[k] = op
            self.rdall[k] = []
        op.idx = len(self.allops)
        self.allops.append(op)
        self.ops[eng].append(op)
        return op

    def schedule(self):
        import heapq
        ops = self.allops
        for op in ops:
            op.succ = []
            op.nd = 0
        for op in ops:
            for d in op.deps:
                d.succ.append((op, 0))
                op.nd += 1
            for d in op.odeps:
                if d not in op.deps:
                    d.succ.append((op, 1))
                    op.nd += 1
        import bisect
        rt = [0.0] * len(ops)
        cand = {e: [] for e in ENGS}
        free = {e: 0.0 for e in ENGS}
        for op in ops:
            if op.nd == 0:
                cand[op.eng].append(op.idx)
        order = {e: [] for e in ENGS}
        W = 24
        remaining = len(ops)
        while remaining:
            best = None
            for e in ENGS:
                h = cand[e]
                if not h:
                    continue
                fe = free[e] + 0.05
                ch = None
                n = min(W, len(h))
                for pos in range(n):
                    ix = h[pos]
                    if rt[ix] <= fe:
                        ch = (free[e], ix, pos)
                        break
                if ch is None:
                    pos = min(range(n), key=lambda p_: (rt[h[p_]], p_))
                    ch = (rt[h[pos]], h[pos], pos)
                if best is None or ch[:2] < best[0][:2]:
                    best = (ch, e)
            (st, ix, pos), e = best
            op = ops[ix]
            del cand[e][pos]
            st = max(st, free[e])
            op.t0 = st
            op.t1 = st + op.lat
            free[e] = st + op.dur
            order[e].append(op)
            remaining -= 1
            for (sx, kind) in op.succ:
                tdep = op.t0 if kind == 1 else op.t1
                if tdep > rt[sx.idx]:
                    rt[sx.idx] = tdep
                sx.nd -= 1
                if sx.nd == 0:
                    bisect.insort(cand[sx.eng], sx.idx)
        self.ops = order
        self.sim_time = max(free.values())
        if os.environ.get("KVERBOSE") == "1":
            busy = {e: sum(o.dur for o in order[e]) for e in ENGS}
            print("phase sim_time us %.0f nops %d" % (self.sim_time, len(ops)), " ".join("%s=%.0f" % (e, busy[e]) for e in ENGS))

    def emit(self):
        nc = self.nc
        if os.environ.get("KNOSCHED") != "1":
            self.schedule()
        for e in ENGS:
            for op in self.ops[e]:
                for d in op.deps:
                    if d.dma is None and not (d.eng == "pe" and op.eng == "pe"):
                        d.inc = True
        for e in ENGS:
            for op in self.ops[e]:
                if op.dma is None and op.inc:
                    self.ecnt[e] += 1
                    op.val = self.ecnt[e]
        final_d = list(self.dcnt)
        used_d = sorted(set(self.dkey.values()))

        def run(ename, eng):
            known = {}
            for op in self.ops[ename]:
                need = {}
                for d in op.deps:
                    if d.dma is not None:
                        key = ("d", d.dma)
                    else:
                        if d.eng == "pe" and ename == "pe":
                            continue
                        key = ("e", d.eng)
                    if d.val > need.get(key, 0):
                        need[key] = d.val
                for key, val in need.items():
                    if known.get(key, 0) >= val:
                        continue
                    known[key] = val
                    sem = self.dsems[key[1]] if key[0] == "d" else self.esems[key[1]]
                    eng.wait_ge(sem, val)
                inst = op.fn(eng)
                if op.dma is not None:
                    inst.then_inc(self.dsems[op.dma], 16)
                elif op.inc:
                    inst.then_inc(self.esems[ename], 1)
            if ename == "sp":
                for si in used_d:
                    eng.wait_ge(self.dsems[si], final_d[si])

        with nc.Block() as block:
            @block.tensor
            def _(e):
                run("pe", e)

            @block.scalar
            def _(e):
                run("act", e)

            @block.vector
            def _(e):
                run("dve", e)

            @block.gpsimd
            def _(e):
                run("pool", e)

            @block.sync
            def _(e):
                run("sp", e)
        nc.all_engine_barrier()
        self.reset_phase()

    @staticmethod
    def _edur(eng, ap):
        f = _fsz(ap)
        if eng == "act":
            return 0.22 + f / 1400.0
        if eng == "dve":
            return 0.12 + f / 960.0
        return 0.3 + f / 420.0

    def mm(self, out, lhsT, rhs, start, stop, r, w):
        self.add("pe", lambda e: e.matmul(out, lhsT=lhsT, rhs=rhs, start=start, stop=stop,
                                          skip_group_check=True), r, w, dur=0.03 + _fsz(rhs) / 2300.0)

    def tr(self, out, in_, ident, r, w):
        self.add("pe", lambda e: e.transpose(out, in_, ident), r, w, dur=0.14)

    def actf(self, out, in_, func, r, w, bias=None, scale=None, eng="act"):
        kw = {}
        if bias is not None:
            kw["bias"] = bias
        if scale is not None:
            kw["scale"] = scale
        self.add(eng, lambda e: e.activation(out=out, in_=in_, func=func, **kw), r, w, dur=self._edur(eng, out))

    def copy(self, eng, out, in_, r, w):
        if eng == "act":
            self.add(eng, lambda e: e.activation(out=out, in_=in_, func=AF.Copy), r, w, dur=self._edur(eng, out))
        else:
            self.add(eng, lambda e: e.tensor_copy(out=out, in_=in_), r, w, dur=self._edur(eng, out))

    def tt(self, eng, out, in0, in1, op, r, w):
        self.add(eng, lambda e: e.tensor_tensor(out=out, in0=in0, in1=in1, op=op), r, w, dur=self._edur(eng, out))

    def ts(self, eng, out, in0, s1, s2, op0, op1, r, w):
        if op1 is None:
            self.add(eng, lambda e: e.tensor_scalar(out=out, in0=in0, scalar1=s1, scalar2=None, op0=op0), r, w,
                     dur=self._edur(eng, out))
        else:
            self.add(eng, lambda e: e.tensor_scalar(out=out, in0=in0, scalar1=s1, scalar2=s2,
                                                    op0=op0, op1=op1), r, w, dur=self._edur(eng, out))

    def stt(self, eng, out, in0, scalar, in1, op0, op1, r, w):
        self.add(eng, lambda e: e.scalar_tensor_tensor(out=out, in0=in0, scalar=scalar, in1=in1,
                                                       op0=op0, op1=op1), r, w, dur=self._edur(eng, out))

    def memset(self, eng, ap, val, w):
        self.add(eng, lambda e: e.memset(ap, val), (), w, dur=self._edur(eng, ap))

    def dma(self, eng, out, in_, key, r, w):
        nbytes = _fsz(out) * int(out.shape[0]) * (2 if out.dtype == BF16 else 4)
        self.add(eng, lambda e: e.dma_start(out=out, in_=in_), r, w, dma=key,
                 dur=(1.0 if eng == "pool" else 0.1), lat=2.2 + nbytes / 120e3)

    def layernorm(self, x, o, g, b, width, tmp, xk, ok, gk, tk, eng2="dve"):
        self.lncnt = getattr(self, "lncnt", 0) + 1
        si = self.lncnt % len(tmp)
        tk = tk + str(si)
        eps = tmp[0]["eps"]
        tmp = tmp[si]
        st, mv, rstd = tmp["st"], tmp["mv"], tmp["rstd"]
        nch = width // 512
        for c in range(nch):
            self.add("dve", (lambda c: lambda e: e.bn_stats(out=st[:, c * 6:(c + 1) * 6],
                                                             in_=x[:, c * 512:(c + 1) * 512]))(c),
                     xk, [tk + "st"])
        self.add("dve", lambda e: e.bn_aggr(out=mv[:, 0:2], in_=st[:, 0:6 * nch]), [tk + "st"], [tk + "mv"])
        lnv, nmr = tmp["lnv"], tmp["nmr"]
        self.actf(lnv[:, 0:1], mv[:, 1:2], AF.Ln, [tk + "mv"], [tk + "lnv"], bias=eps[:, 0:1])
        self.actf(rstd[:, 0:1], lnv[:, 0:1], AF.Exp, [tk + "lnv"], [tk + "rs"], scale=-0.5)
        self.ts("dve", nmr[:, 0:1], mv[:, 0:1], rstd[:, 0:1], -1.0, ALU.mult, ALU.mult, [tk + "mv", tk + "rs"], [tk + "nmr"])
        self.actf(o, x, AF.Identity, list(xk) + [tk + "nmr", tk + "rs"], ok, bias=nmr[:, 0:1], scale=rstd[:, 0:1])
        self.tt(eng2, o, o, g, ALU.mult, list(ok) + list(gk), ok)
        self.tt(eng2, o, o, b, ALU.add, list(ok) + list(gk), ok)


def _consts(parity):
    k = np.arange(128)
    c = {}
    c["ident_f"] = np.eye(128, dtype=np.float32)
    c["ident_b"] = np.eye(128, dtype=np.float32).astype(ml_dtypes.bfloat16)
    c["ut"] = (k[:, None] <= k[None, :]).astype(np.float32)
    c["ones_f"] = np.ones((128, 128), np.float32)
    c["ones_b"] = np.ones((128, 128), np.float32).astype(ml_dtypes.bfloat16)
    c["cmaskT"] = ((k[:, None] // 64) <= (k[None, :] // 64)).astype(np.float32)
    causal = np.where(k[:, None] <= k[None, :], 0.0, NEG).astype(np.float32)
    zeros = np.zeros((128, 128), np.float32)
    full = np.full((128, 128), NEG, np.float32)
    if parity == 0:
        mE, mO = causal, full
    else:
        mE, mO = zeros, causal
    c["maskE"] = mE.astype(ml_dtypes.bfloat16)
    c["maskO"] = mO.astype(ml_dtypes.bfloat16)
    c["pE"] = np.full((128, 1), 1.0 - parity, np.float32)
    c["pO"] = np.full((128, 1), float(parity), np.float32)
    c["trash"] = np.broadcast_to((NSLOT + np.arange(128)).astype(np.float32)[:, None], (128, NE)).copy()
    c["ecap"] = np.broadcast_to((np.arange(NE) * CAP).astype(np.float32)[None, :], (128, NE)).copy()
    return c


def build_program(debug=False, stop_after=99):
    nc = bass.Bass("TRN2", target_bir_lowering=False)
    ins = {}

    def din(name, shape, dt=F32):
        ins[name] = nc.dram_tensor(name, list(shape), dt, kind="ExternalInput").ap()
        return ins[name]

    skind = "ExternalOutput" if debug else "Internal"

    def dscr(name, shape, dt):
        return nc.dram_tensor(name, list(shape), dt, kind=skind).ap()

    x_all = din("x_all", [SEQ, D])
    x_own = din("x_own", [NOB * 128, D])
    mem = din("mem", [256, D])
    lnin_g = din("lnin_g", [128, D]); lnin_b = din("lnin_b", [128, D])
    w_in = din("w_in", [D, 4616])
    bfor = din("bfor", [128, 8])
    lnv_g = din("lnv_g", [128, 512]); lnv_b = din("lnv_b", [128, 512])
    wspT = din("wspT", [8, 128, 128])
    bspf = din("bspf", [128, 512])
    w_ba = din("w_ba", [512, D]); w_bb = din("w_bb", [512, D]); w_out = din("w_out", [D, D])
    ln1_g = din("ln1_g", [128, D]); ln1_b = din("ln1_b", [128, D])
    lnm_g = din("lnm_g", [128, D]); lnm_b = din("lnm_b", [128, D])
    w_mq = din("w_mq", [D, D]); w_mkv = din("w_mkv", [D, 2 * D]); w_mo = din("w_mo", [D, D])
    ln2_g = din("ln2_g", [128, D]); ln2_b = din("ln2_b", [128, D])
    w_r = din("w_r", [D, NE]); b_r = din("b_r", [128, NE])
    w_gu = din("w_gu", [NE, D, 2 * D]); b_gu = din("b_gu", [NE, 128, 16])
    w_d = din("w_d", [NE, D, D]); b_d = din("b_d", [NE, D])
    ln3_g = din("ln3_g", [128, D]); ln3_b = din("ln3_b", [128, D])
    ident_f_d = din("ident_f", [128, 128]); ident_b_d = din("ident_b", [128, 128], BF16)
    ut_d = din("ut", [128, 128]); ones_f_d = din("ones_f", [128, 128]); ones_b_d = din("ones_b", [128, 128], BF16)
    cmaskT_d = din("cmaskT", [128, 128])
    maskE_d = din("maskE", [128, 128], BF16); maskO_d = din("maskO", [128, 128], BF16)
    pE_d = din("pE", [128, 1]); pO_d = din("pO", [128, 1]); ecap_d = din("ecap", [128, NE]); trash_d = din("trash", [128, NE])

    out_d = nc.dram_tensor("out", [NOB * 128, D], F32, kind="ExternalOutput").ap()

    Kt_d = dscr("Kt_d", [8, 64, SEQ], BF16)
    V_d = dscr("V_d", [8, 128, NB, 65], BF16)
    Qt_d = dscr("Qt_d", [8, 64, NOB * 128], BF16)
    Ca_d = dscr("Ca_d", [8, NOB * 128], BF16)
    yaT_d = dscr("yaT_d", [NG, 128, 4, 512], BF16)
    ybT_d = dscr("ybT_d", [NG, 128, 4, 512], BF16)
    sg_d = dscr("sg_d", [NG, 128, 16, 512], BF16)
    xn_d = dscr("xn_d", [NOB * 128, D], F32)
    x2_d = dscr("x2_d", [NOB * 128, D], F32)
    xg_d = dscr("xg_d", [NSLOT + 128, D], BF16)
    ye_d = dscr("ye_d", [NSLOT + 128, D], BF16)
    cnt_d = dscr("cnt_d", [128, NE], F32) if debug else None

    es = ExitStack()
    with es:
        esems = {e: es.enter_context(nc.semaphore("sem_" + e)) for e in ("pe", "act", "dve", "pool")}
        dsems = [es.enter_context(nc.semaphore("dsem%d" % i)) for i in range(40)]
        P = Prog(nc, esems, dsems)

        uid = [0]

        def sb(name, shape, dt, stack=es):
            uid[0] += 1
            return stack.enter_context(nc.sbuf_tensor("s%d_%s" % (uid[0], name), list(shape), dt))

        psf = [es.enter_context(nc.psum_tensor("psf%d" % i, [128, 512], F32)) for i in range(6)]
        psb = [es.enter_context(nc.psum_tensor("psb%d" % i, [128, 1024], BF16)) for i in range(2)]

        ident_f = sb("ident_f", [128, 128], F32); ident_b = sb("ident_b", [128, 128], BF16)
        ut = sb("ut", [128, 128], F32); ones_f = sb("ones_f", [128, 128], F32); ones_b = sb("ones_b", [128, 128], BF16)
        maskE = sb("maskE", [128, 128], BF16); maskO = sb("maskO", [128, 128], BF16)
        pE = sb("pE", [128, 1], F32); pO = sb("pO", [128, 1], F32)
        negc_all = sb("negc_all", [128, NB, 8], F32)
        negc_own = sb("negc_own", [128, NOB, 8], F32)
        gw4 = sb("gw4", [128, NOB, 4], F32)
        dest4 = sb("dest4", [128, NOB, 4], I32)
        kmT = sb("kmT", [128, 8, 256], BF16)
        vmem = sb("vmem", [128, 2, 1024], BF16)
        lnt = [{"st": sb("ln_st", [128, 12], F32), "mv": sb("ln_mv", [128, 2], F32), "rstd": sb("ln_rstd", [128, 1], F32),
                "lnv": sb("ln_lnv", [128, 1], F32), "nmr": sb("ln_nmr", [128, 1], F32), "eps": sb("ln_eps", [128, 1], F32)}
               for _ in range(3)]
        P.memset("dve", lnt[0]["eps"][:], EPS, ["lneps"])

        ph12 = ExitStack()
        wu = sb("wu", [128, 8, 512], BF16, ph12); wvg = sb("wvg", [128, 8, 512], BF16, ph12)
        wq = sb("wq", [128, 8, 512], BF16, ph12); wg = sb("wg", [128, 8, 2048], BF16, ph12)
        with ExitStack() as ph:
            def psb_(name, shape, dt):
                return sb(name, shape, dt, ph)
            for i, (t, d_) in enumerate([(ident_f, ident_f_d), (ident_b, ident_b_d), (ut, ut_d), (ones_f, ones_f_d),
                                         (ones_b, ones_b_d), (maskE, maskE_d), (maskO, maskO_d), (pE, pE_d), (pO, pO_d)]):
                P.dma("sp", t[:], d_, "const", [], ["const"])
            lng = psb_("lng", [128, D], F32); lnb = psb_("lnb", [128, D], F32)
            P.dma("sp", lng[:], lnin_g, "lnpk", [], ["lnp"])
            P.dma("sp", lnb[:], lnin_b, "lnpk", [], ["lnp"])
            bfo = psb_("bfo", [128, 8], F32)
            P.dma("sp", bfo[:], bfor, "lnpk", [], ["lnp"])
            wk = psb_("wk", [128, 8, 512], BF16); wv = psb_("wv", [128, 8, 512], BF16); wf = psb_("wf", [128, 8, 8], BF16)
            P.dma("pool", wk[:], w_in[:, 1536:2048].rearrange("(kc p) c -> p kc c", p=128), "wld0", [], ["wk"])
            P.dma("pool", wv[:], w_in[:, 2048:2560].rearrange("(kc p) c -> p kc c", p=128), "wld1", [], ["wv"])
            P.dma("pool", wf[:], w_in[:, 2560:2568].rearrange("(kc p) c -> p kc c", p=128), "wld2", [], ["wf"])
            P.dma("pool", wu[:], w_in[:, 0:512].rearrange("(kc p) c -> p kc c", p=128), "wldu", [], ["wu"])
            P.dma("pool", wvg[:], w_in[:, 512:1024].rearrange("(kc p) c -> p kc c", p=128), "wldvg", [], ["wvg"])
            P.dma("pool", wq[:], w_in[:, 1024:1536].rearrange("(kc p) c -> p kc c", p=128), "wldq", [], ["wq"])
            for i in range(4):
                P.dma("pool", wg[:, :, i * 512:(i + 1) * 512],
                      w_in[:, 2568 + i * 512:2568 + (i + 1) * 512].rearrange("(kc p) c -> p kc c", p=128),
                      "wldg", [], [("wg", i)])
            xt = [psb_("xt%d" % i, [128, D], F32) for i in range(4)]
            xnb = [psb_("xnb%d" % i, [128, D], BF16) for i in range(4)]
            xnT = [psb_("xnT%d" % i, [128, 8, 512], BF16) for i in range(2)]
            ktst = [psb_("ktst%d" % i, [64, 8, 512], BF16) for i in range(2)]
            vst = [psb_("vst%d" % i, [128, 8, 4, 65], BF16) for i in range(2)]
            Cb = psb_("Cb", [128, 8], F32)
            fl = psb_("fl", [128, 8], F32); fe = psb_("fe", [128, 8], F32); fsp = psb_("fsp", [128, 8], F32)
            P.memset("dve", Cb[:], 0.0, ["Cb"])
            for i in range(2):
                P.memset("pool", vst[i][:, :, :, 64:65], 1.0, [("vst", i)])

            nblk_kv = NB if stop_after >= 1 else 8
            xcount = 0

            def load_x(src, blk, cnt):
                s = cnt % 4
                P.dma("sp", xt[s][:], src[blk * 128:(blk + 1) * 128, :], "xt%d" % s, [], [("xt", s)])

            def ln_and_transpose(cnt, bi, gslot, resid=None):
                s = cnt % 4
                nbs = cnt % 4
                P.layernorm(xt[s][:], xt[s][:], lng[:], lnb[:], D, lnt, [("xt", s)], [("xt", s)], ["lnp"], "l")
                if resid is not None:
                    resid(s)
                P.copy("act", xnb[nbs][:], xt[s][:], [("xt", s)], [("xnb", nbs)])
                pb = psb[cnt % 2]
                for kc in range(8):
                    P.tr(pb[:, kc * 128:(kc + 1) * 128], xnb[nbs][:, kc * 128:(kc + 1) * 128], ident_b[:],
                         [("xnb", nbs), "const"], [("psb", cnt % 2)])
                P.copy("dve", xnT[gslot][:, :, bi * 128:(bi + 1) * 128],
                       pb[:].rearrange("p (k t) -> p k t", k=8), [("psb", cnt % 2)], [("xnT", gslot, bi)])

            ngrp = nblk_kv // 4
            load_x(x_all, 0, 0)
            load_x(x_all, 1, 1)
            load_x(x_all, 2, 2)
            cnt = 0
            for g in range(ngrp):
                gs = g % 2
                for bi in range(4):
                    blk = g * 4 + bi
                    if blk + 3 < nblk_kv:
                        load_x(x_all, blk + 3, cnt + 3)
                    ln_and_transpose(cnt, bi, gs)
                    cnt += 1
                xk = [("xnT", gs, bi) for bi in range(4)]
                for h in range(8):
                    pk = psf[h % 2]
                    for kc in range(8):
                        P.mm(pk[0:64, :], wk[:, kc, h * 64:(h + 1) * 64], xnT[gs][:, kc, :], kc == 0, kc == 7,
                             xk + ["wk"], [("psf", h % 2)])
                    P.copy("act" if h % 2 == 0 else "dve", ktst[gs][:, h, :], pk[0:64, :], [("psf", h % 2)], [("ktst", gs)])
                P.dma("sp", Kt_d[:, :, g * 512:(g + 1) * 512].rearrange("h d t -> d h t"), ktst[gs][:], "kst%d" % gs,
                      [("ktst", gs)], [("Kt_d", g)])
                for bi in range(4):
                    pv = psf[2 + bi % 2]
                    for kc in range(8):
                        P.mm(pv[:], xnT[gs][:, kc, bi * 128:(bi + 1) * 128], wv[:, kc, :], kc == 0, kc == 7,
                             [("xnT", gs, bi), "wv"], [("psf", 2 + bi % 2)])
                    P.copy("act" if bi % 2 == 0 else "dve", vst[gs][:, :, bi, 0:64],
                           pv[:].rearrange("p (h d) -> p h d", h=8), [("psf", 2 + bi % 2)], [("vst", gs)])
                P.dma("sp", V_d[:, :, g * 4:(g + 1) * 4, :].rearrange("h p b c -> p h b c"), vst[gs][:], "vst%d" % gs,
                      [("vst", gs)], [("V_d", g)])
                for bi in range(4):
                    blk = g * 4 + bi
                    pf = psf[4]
                    for kc in range(8):
                        P.mm(pf[:, 0:8], xnT[gs][:, kc, bi * 128:(bi + 1) * 128], wf[:, kc, :], kc == 0, kc == 7,
                             [("xnT", gs, bi), "wf"], [("psf", 4)])
                    P.tt("dve", fl[:], pf[:, 0:8], bfo[:], ALU.add, [("psf", 4), "lnp"], ["fl"])
                    P.actf(fe[:], fl[:], AF.Exp, ["fl"], ["fe"], scale=-1.0)
                    P.actf(fsp[:], fe[:], AF.Ln, ["fe"], ["fsp"], bias=1.0)
                    pc = psf[5]
                    P.mm(pc[:, 0:8], ut[:], fsp[:], True, True, ["fsp", "const"], [("psf", 5)])
                    P.mm(pc[:, 8:16], ones_f[:], fsp[:], True, True, ["fsp", "const"], [("psf", 5)])
                    P.tt("dve", negc_all[:, blk, :], pc[:, 0:8], Cb[:], ALU.add, [("psf", 5), "Cb"], [("negc", blk)])
                    P.tt("dve", Cb[:], pc[:, 8:16], Cb[:], ALU.add, [("psf", 5), "Cb"], ["Cb"])
            nv = negc_all[:].rearrange("p (m two) h -> p m two h", two=2)
            allneg = [("negc", b) for b in range(nblk_kv)]
            P.ts("dve", negc_own[:], nv[:, :, 0, :], pE[:, 0:1], None, ALU.mult, None, allneg + ["const"], ["negc_own"])
            P.stt("dve", negc_own[:], nv[:, :, 1, :], pO[:, 0:1], negc_own[:], ALU.mult, ALU.add,
                  allneg + ["const", "negc_own"], ["negc_own"])
            cst = psb_("cst", [8, NOB * 128], BF16)
            for m4 in range(NOB // 4):
                pt_ = psf[m4 % 2]
                for j in range(4):
                    m = m4 * 4 + j
                    P.tr(pt_[0:8, j * 128:(j + 1) * 128], negc_own[:, m, :], ident_f[:], ["negc_own", "const"],
                         [("psf", m4 % 2)])
                P.actf(cst[:, m4 * 512:(m4 + 1) * 512], pt_[0:8, :], AF.Copy, [("psf", m4 % 2)], ["cst"], scale=-8.0)
            P.dma("sp", Ca_d, cst[:], "cst", ["cst"], ["Ca_d"])
            P.emit()

        if stop_after < 2:
            return nc, ins

        with ExitStack() as ph:
            def psb_(name, shape, dt):
                return sb(name, shape, dt, ph)
            lng = psb_("lng", [128, D], F32); lnb = psb_("lnb", [128, D], F32)
            P.dma("sp", lng[:], lnin_g, "const", [], ["lnp"])
            P.dma("sp", lnb[:], lnin_b, "const", [], ["lnp"])
            lvg = psb_("lvg", [128, 512], F32); lvb = psb_("lvb", [128, 512], F32); bsp = psb_("bsp", [128, 512], F32)
            P.dma("sp", lvg[:], lnv_g, "const", [], ["lnp"])
            P.dma("sp", lvb[:], lnv_b, "const", [], ["lnp"])
            P.dma("sp", bsp[:], bspf, "const", [], ["lnp"])
            cmT = psb_("cmT", [128, 128], F32)
            P.dma("sp", cmT[:], cmaskT_d, "const", [], ["lnp"])
            wsf = psb_("wsf", [128, 8, 128], F32); wsb = psb_("wsb", [128, 8, 128], BF16)
            P.dma("sp", wsf[:], wspT.rearrange("g s t -> s g t"), "const", [], ["wsf"])
            for gi in range(8):
                P.tt("dve", wsb[:, gi, :], wsf[:, gi, :], cmT[:], ALU.mult, ["wsf", "lnp"], ["wsb"])
            xt = [psb_("xt%d" % i, [128, D], F32) for i in range(4)]
            xnb = [psb_("xnb%d" % i, [128, D], BF16) for i in range(4)]
            xnT = [psb_("xnT%d" % i, [128, 8, 512], BF16) for i in range(2)]
            qtst = [psb_("qtst%d" % i, [64, 8, 512], BF16) for i in range(2)]
            sgst = [psb_("sgst%d" % i, [128, 16, 512], BF16) for i in range(2)]
            yaTs = [psb_("yaTs%d" % i, [128, 4, 512], BF16) for i in range(2)]
            u_sb = psb_("u_sb", [128, 512], F32); vgl = psb_("vgl", [128, 512], F32)
            vln = psb_("vln", [128, 512], BF16); tmpa = psb_("tmpa", [128, 512], F32); ya = psb_("ya", [128, 512], BF16)

            ng2 = NG if stop_after >= 2 else 1
            nblk = ng2 * 4
            load_x(x_own, 0, 0)
            load_x(x_own, 1, 1)
            load_x(x_own, 2, 2)
            cnt = 0
            for g in range(ng2):
                gs = g % 2
                for bi in range(4):
                    blk = g * 4 + bi
                    if blk + 3 < nblk:
                        load_x(x_own, blk + 3, cnt + 3)

                    def resid(s, blk=blk):
                        P.dma("sp", xn_d[blk * 128:(blk + 1) * 128, :], xt[s][:], "xnst%d" % s, [("xt", s)], [("xn_d", blk)])
                    ln_and_transpose(cnt, bi, gs, resid)
                    cnt += 1
                    xkb = [("xnT", gs, bi)]
                    pu = psf[0]
                    for kc in range(8):
                        P.mm(pu[:], xnT[gs][:, kc, bi * 128:(bi + 1) * 128], wu[:, kc, :], kc == 0, kc == 7,
                             xkb + ["wu"], [("psf", 0)])
                    P.actf(u_sb[:], pu[:], AF.Gelu, [("psf", 0)], ["u_sb"])
                    pv = psf[1]
                    for kc in range(8):
                        P.mm(pv[:], xnT[gs][:, kc, bi * 128:(bi + 1) * 128], wvg[:, kc, :], kc == 0, kc == 7,
                             xkb + ["wvg"], [("psf", 1)])
                    P.actf(vgl[:], pv[:], AF.Gelu, [("psf", 1)], ["vgl"])
                    P.layernorm(vgl[:], vgl[:], lvg[:], lvb[:], 512, lnt, ["vgl"], ["vgl"], ["lnp"], "l")
                    P.copy("pool", vln[:], vgl[:], ["vgl"], ["vln"])
                    pm = psf[2]
                    for gi in range(8):
                        P.mm(pm[:, gi * 64:(gi + 1) * 64], wsb[:, gi, :], vln[:, gi * 64:(gi + 1) * 64], True, True,
                             ["vln", "wsb"], [("psf", 2)])
                    P.tt("dve", tmpa[:], pm[:], bsp[:], ALU.add, [("psf", 2), "lnp"], ["tmpa"])
                    P.tt("pool", ya[:], tmpa[:], u_sb[:], ALU.mult, ["tmpa", "u_sb"], ["ya"])
                    pb = psb[cnt % 2]
                    for kc in range(4):
                        P.tr(pb[:, kc * 128:(kc + 1) * 128], ya[:, kc * 128:(kc + 1) * 128], ident_b[:],
                             ["ya", "const"], [("psb", cnt % 2)])
                    P.copy("dve", yaTs[gs][:, :, bi * 128:(bi + 1) * 128],
                           pb[:, 0:512].rearrange("p (k t) -> p k t", k=4), [("psb", cnt % 2)], [("yaTs", gs)])
                xk = [("xnT", gs, bi) for bi in range(4)]
                P.dma("sp", yaT_d[g], yaTs[gs][:], "yaTs%d" % gs, [("yaTs", gs)], [("yaT_d", g)])
                for h in range(8):
                    pk = psf[3 + h % 2]
                    for kc in range(8):
                        P.mm(pk[0:64, :], wq[:, kc, h * 64:(h + 1) * 64], xnT[gs][:, kc, :], kc == 0, kc == 7,
                             xk + ["wq"], [("psf", 3 + h % 2)])
                    P.copy("dve" if h % 2 == 0 else "act", qtst[gs][:, h, :], pk[0:64, :], [("psf", 3 + h % 2)],
                           [("qtst", gs)])
                P.dma("sp", Qt_d[:, :, g * 512:(g + 1) * 512].rearrange("h d t -> d h t"), qtst[gs][:], "qst%d" % gs,
                      [("qtst", gs)], [("Qt_d", g)])
                for cc in range(16):
                    pg = psf[3 + cc % 3]
                    for kc in range(8):
                        P.mm(pg[:], wg[:, kc, cc * 128:(cc + 1) * 128], xnT[gs][:, kc, :], kc == 0, kc == 7,
                             xk + ["wg"], [("psf", 3 + cc % 3)])
                    P.actf(sgst[gs][:, cc, :], pg[:], AF.Sigmoid, [("psf", 3 + cc % 3)], [("sgst", gs)])
                P.dma("sp", sg_d[g], sgst[gs][:], "sgst%d" % gs, [("sgst", gs)], [("sg_d", g)])
            P.emit()
        ph12.close()
        if stop_after < 3:
            return nc, ins

        ph45 = ExitStack()
        wba = sb("wba", [128, 4, D], BF16, ph45); wbb = sb("wbb", [128, 4, D], BF16, ph45)
        wout = sb("wout", [128, 8, D], BF16, ph45); wmq = sb("wmq", [128, 8, D], BF16, ph45); wmo = sb("wmo", [128, 8, D], BF16, ph45)
        with ExitStack() as ph:
            def psb_(name, shape, dt):
                return sb(name, shape, dt, ph)
            for i in range(2):
                P.dma("pool", wba[:, :, i * 512:(i + 1) * 512], w_ba[:, i * 512:(i + 1) * 512].rearrange("(kc p) c -> p kc c", p=128), "w0", [], [("wba", i)])
                P.dma("pool", wbb[:, :, i * 512:(i + 1) * 512], w_bb[:, i * 512:(i + 1) * 512].rearrange("(kc p) c -> p kc c", p=128), "w1", [], [("wbb", i)])
                P.dma("pool", wout[:, :, i * 512:(i + 1) * 512], w_out[:, i * 512:(i + 1) * 512].rearrange("(kc p) c -> p kc c", p=128), "w2", [], [("wout", i)])
                P.dma("pool", wmq[:, :, i * 512:(i + 1) * 512], w_mq[:, i * 512:(i + 1) * 512].rearrange("(kc p) c -> p kc c", p=128), "w3", [], [("wmq", i)])
                P.dma("pool", wmo[:, :, i * 512:(i + 1) * 512], w_mo[:, i * 512:(i + 1) * 512].rearrange("(kc p) c -> p kc c", p=128), "w4", [], [("wmo", i)])
            P.latadd = 0.0
            ka = [psb_("ka%d" % i, [65, SEQ], BF16) for i in range(2)]
            va = [psb_("va%d" % i, [128, NB, 65], BF16) for i in range(2)]
            qa = [psb_("qa%d" % i, [65, 512], BF16) for i in range(2)]
            pts = [psb_("pt%d" % i, [128, 512], BF16) for i in range(4)]
            yb = psb_("yb", [128, 4, 512], BF16)
            ybTs = [psb_("ybTs%d" % i, [128, 4, 512], BF16) for i in range(2)]
            rs = psb_("rs", [128, 4], F32)
            for i in range(2):
                P.memset("pool", ka[i][64:65, :], 1.0, [("karow", i)])
            ng4 = NG if stop_after >= 4 else 1
            nit = ng4 * 8

            def att_load(it):
                g, h = it // 8, it % 8
                s = it % 2
                nkb = 8 * g + 8
                P.dma("sp", ka[s][0:64, 0:nkb * 128], Kt_d[h, :, 0:nkb * 128], "ka%d" % s, [], [("ka", s)])
                P.dma("sp", va[s][:, 0:nkb, :], V_d[h, :, 0:nkb, :], "va%d" % s, [], [("va", s)])
                P.dma("sp", qa[s][0:64, :], Qt_d[h, :, g * 512:(g + 1) * 512], "qa%d" % s, [], [("qa", s)])
                P.dma("sp", qa[s][64:65, :], Ca_d[h:h + 1, g * 512:(g + 1) * 512], "qa%d" % s, [], [("qa", s)])

            att_load(0)
            pcount = 0
            for it in range(nit):
                g, h = it // 8, it % 8
                s = it % 2
                gs = g % 2
                nkb = 8 * g + 8
                if it + 1 < nit:
                    att_load(it + 1)
                ob = 3 + it % 2
                O = psf[ob]
                O3 = O[:, 0:260].rearrange("p (q c) -> p q c", c=65)

                def qmin(j):
                    return max(0, (j - 8 * g - 1 + 1) // 2)

                def s_mm(j):
                    a = qmin(j) * 128
                    S = psf[j % 3]
                    msk = []
                    for qi in range(qmin(j), 4):
                        if j == 8 * g + 2 * qi:
                            msk.append((qi, maskE))
                        elif j == 8 * g + 2 * qi + 1:
                            msk.append((qi, maskO))
                    P.mm(S[:, a:512], ka[s][0:65, j * 128:(j + 1) * 128], qa[s][0:65, a:512], True, len(msk) == 0,
                         [("ka", s), ("karow", s), ("qa", s)], [("psf", j % 3)])
                    for n, (qi, mk) in enumerate(msk):
                        P.mm(S[:, qi * 128:(qi + 1) * 128], ident_b[:], mk[:], False, n == len(msk) - 1,
                             [], [("psf", j % 3)])

                s_mm(0)
                if nkb > 1:
                    s_mm(1)
                for j in range(nkb):
                    a = qmin(j) * 128
                    pi = pcount % 4
                    pcount += 1
                    P.actf(pts[pi][:, a:512], psf[j % 3][:, a:512], AF.Exp, [("psf", j % 3)], [("pt", pi)],
                           bias=negc_all[:, j, h:h + 1], scale=0.125)
                    if j + 2 < nkb:
                        s_mm(j + 2)
                    for qi in range(qmin(j), 4):
                        P.mm(O3[:, qi, :], pts[pi][:, qi * 128:(qi + 1) * 128], va[s][:, j, :],
                             (j == 0 and qi == 0), j == 8 * g + 2 * qi + 1, [("pt", pi), ("va", s)], [("psf", ob)])
                P.add("dve", lambda e, O3=O3: e.reciprocal(out=rs[:, 0:4], in_=O3[:, :, 64]), [("psf", ob)], ["rs"])
                for qi in range(4):
                    P.ts("dve", yb[:, qi, h * 64:(h + 1) * 64], O3[:, qi, 0:64], rs[:, qi:qi + 1], 1.0, ALU.mult, ALU.mult,
                         [("psf", ob), "rs"], [("yb", qi)])
                if h == 7:
                    for qi in range(4):
                        pb = psb[qi % 2]
                        for kc in range(4):
                            P.tr(pb[:, kc * 128:(kc + 1) * 128], yb[:, qi, kc * 128:(kc + 1) * 128], ident_b[:],
                                 [("yb", qi)], [("psb", qi % 2)])
                        P.copy("dve", ybTs[gs][:, :, qi * 128:(qi + 1) * 128],
                               pb[:, 0:512].rearrange("p (k t) -> p k t", k=4), [("psb", qi % 2)], [("ybTs", gs)])
                    P.dma("sp", ybT_d[g], ybTs[gs][:], "ybTs%d" % gs, [("ybTs", gs)], [("ybT_d", g)])
            P.emit()
        if stop_after < 5:
            return nc, ins

        P.latadd = 0.2
        with ExitStack() as ph:
            def psb_(name, shape, dt):
                return sb(name, shape, dt, ph)
            lmg = psb_("lmg", [128, D], F32); lmb = psb_("lmb", [128, D], F32)
            P.dma("sp", lmg[:], lnm_g, "c1", [], ["lnp"])
            P.dma("sp", lmb[:], lnm_b, "c2", [], ["lnp2"])
            wmkv = psb_("wmkv", [128, 8, 2048], BF16)
            for i in range(4):
                P.dma("pool", wmkv[:, :, i * 512:(i + 1) * 512],
                      w_mkv[:, i * 512:(i + 1) * 512].rearrange("(kc p) c -> p kc c", p=128), "w%d" % i, [], [("wmkv", i)])
            wmk = [("wmkv", i) for i in range(4)]
            mt = [psb_("mt%d" % i, [128, D], F32) for i in range(2)]
            mtb = [psb_("mtb%d" % i, [128, D], BF16) for i in range(2)]
            memT = psb_("memT", [128, 8, 256], BF16)
            for i in range(2):
                P.dma("sp", mt[i][:], mem[i * 128:(i + 1) * 128, :], "mt%d" % i, [], [("mt", i)])
                P.layernorm(mt[i][:], mt[i][:], lmg[:], lmb[:], D, lnt, [("mt", i)], [("mt", i)], ["lnp", "lnp2"], "l")
                P.copy("act", mtb[i][:], mt[i][:], [("mt", i)], [("mtb", i)])
                pb = psb[i]
                for kc in range(8):
                    P.tr(pb[:, kc * 128:(kc + 1) * 128], mtb[i][:, kc * 128:(kc + 1) * 128], ident_b[:], [("mtb", i)], [("psb", i)])
                P.copy("dve", memT[:, :, i * 128:(i + 1) * 128], pb[:].rearrange("p (k t) -> p k t", k=8), [("psb", i)], ["memT"])
            for cc in range(8):
                pk = psf[cc % 2]
                for kc in range(8):
                    P.mm(pk[:, 0:256], wmkv[:, kc, cc * 128:(cc + 1) * 128], memT[:, kc, :], kc == 0, kc == 7,
                         wmk + ["memT"], [("psf", cc % 2)])
                P.copy("act" if cc % 2 == 0 else "dve", kmT[:, cc, :], pk[:, 0:256], [("psf", cc % 2)], ["kmT"])
            for mc in range(2):
                for half in range(2):
                    pv = psf[2 + half]
                    for kc in range(8):
                        P.mm(pv[:], memT[:, kc, mc * 128:(mc + 1) * 128], wmkv[:, kc, 1024 + half * 512:1024 + (half + 1) * 512],
                             kc == 0, kc == 7, wmk + ["memT"], [("psf", 2 + half)])
                    P.copy("act" if half == 0 else "dve", vmem[:, mc, half * 512:(half + 1) * 512], pv[:], [("psf", 2 + half)], ["vmem"])
            P.emit()

        with ExitStack() as ph:
            def psb_(name, shape, dt):
                return sb(name, shape, dt, ph)
            l1g = psb_("l1g", [128, D], F32); l1b = psb_("l1b", [128, D], F32)
            l2g = psb_("l2g", [128, D], F32); l2b = psb_("l2b", [128, D], F32)
            for i, (t, d_) in enumerate([(l1g, ln1_g), (l1b, ln1_b), (l2g, ln2_g), (l2b, ln2_b)]):
                P.dma("sp", t[:], d_, "c%d" % i, [], ["lnp%d" % i])
            lnk = ["lnp%d" % i for i in range(4)]
            wr = psb_("wr", [128, 8, NE], F32); br = psb_("br", [128, NE], F32); ecap = psb_("ecap", [128, NE], F32)
            P.dma("sp", wr[:], w_r.rearrange("(kc p) c -> p kc c", p=128), "c4", [], ["wr"])
            P.dma("sp", br[:], b_r, "c5", [], ["br"])
            P.dma("sp", ecap[:], ecap_d, "c6", [], ["ecap"])
            trash = psb_("trash", [128, NE], F32)
            P.dma("sp", trash[:], trash_d, "c6", [], ["ecap"])
            wk2 = lambda n: [(n, 0), (n, 1)]
            yaT_gs = [psb_("yaT_g%d" % i, [128, 4, 512], BF16) for i in range(2)]
            ybT_gs = [psb_("ybT_g%d" % i, [128, 4, 512], BF16) for i in range(2)]
            sg_g = psb_("sg_g", [128, 16, 512], BF16)
            xrs = [psb_("xr%d" % i, [128, 4, D], F32) for i in range(2)]
            mT = psb_("mT", [128, 8, 512], BF16)
            t1 = [psb_("t1_%d" % i, [128, 512], F32) for i in range(2)]
            t2 = [psb_("t2_%d" % i, [128, 512], F32) for i in range(2)]
            xb16 = [psb_("xb16_%d" % i, [128, D], BF16) for i in range(2)]
            x1T = psb_("x1T", [128, 8, 512], BF16); qmT = mT
            pm = [psb_("pm%d" % i, [128, 512], BF16) for i in range(2)]
            oT = x1T; rinv = psb_("rinv", [128, 512], F32)
            x2b = [psb_("x2b%d" % i, [128, D], BF16) for i in range(2)]
            x2Tf = psb_("x2Tf", [128, 8, 128], F32)
            lg = psb_("lg", [128, NE], F32); top8 = psb_("top8", [128, 8], F32); negm = psb_("negm", [128, 1], F32)
            ex = psb_("ex", [128, NE], F32); msk = psb_("msk", [128, NE], F32); em = psb_("em", [128, NE], F32)
            den = psb_("den", [128, 1], F32); rden = psb_("rden", [128, 1], F32); gwd = psb_("gwd", [128, NE], F32)
            dst = psb_("dst", [128, NE], F32); dst2 = psb_("dst2", [128, NE], F32); pen = psb_("pen", [128, NE], F32)
            base = psb_("base", [128, NE], F32); oh = psb_("oh", [128, NE], F32); tm32 = psb_("tm32", [128, NE], F32)
            d4f = psb_("d4f", [128, 4], F32)
            sidx = [psb_("sidx%d" % i, [128, 1], I32) for i in range(8)]
            P.memset("dve", base[:], 0.0, ["base"])
            if os.environ.get("KVERBOSE") == "1":
                print("phase5 sbuf remaining", nc.sbuf_bytes_remaining)
            ng5 = NG if stop_after >= 5 else 1
            bcount = 0
            for g in range(ng5):
                g2 = g % 2
                yaT_g = yaT_gs[g2]; ybT_g = ybT_gs[g2]; xr = xrs[g2]
                XR = "xr%d" % g2
                P.dma("sp", yaT_g[:], yaT_d[g], "l0_%d" % g2, [], ["yaT_g%d" % g2])
                P.dma("sp", ybT_g[:], ybT_d[g], "l1_%d" % g2, [], ["ybT_g%d" % g2])
                P.dma("sp", sg_g[:], sg_d[g], "l2", [], ["sg_g"])
                xrk = [(XR, bi) for bi in range(4)]
                P.dma("sp", xr[:], xn_d[g * 512:(g + 1) * 512, :].rearrange("(b p) d -> p b d", p=128), "l3_%d" % g2, [], xrk)
                for cc in range(8):
                    A = psf[cc % 2]; B = psf[2 + cc % 2]
                    for kc in range(4):
                        P.mm(A[:], wba[:, kc, cc * 128:(cc + 1) * 128], yaT_g[:, kc, :], kc == 0, kc == 3,
                             wk2("wba") + ["yaT_g%d" % g2], [("psf", cc % 2)])
                    for kc in range(4):
                        P.mm(B[:], wbb[:, kc, cc * 128:(cc + 1) * 128], ybT_g[:, kc, :], kc == 0, kc == 3,
                             wk2("wbb") + ["ybT_g%d" % g2], [("psf", 2 + cc % 2)])
                    P.tt("dve", t1[cc % 2][:], A[:], sg_g[:, cc, :], ALU.mult, [("psf", cc % 2), "sg_g"], [("t1", cc % 2)])
                    P.tt("dve", t2[cc % 2][:], B[:], sg_g[:, 8 + cc, :], ALU.mult, [("psf", 2 + cc % 2), "sg_g"], [("t2", cc % 2)])
                    P.tt("pool", mT[:, cc, :], t1[cc % 2][:], t2[cc % 2][:], ALU.add, [("t1", cc % 2), ("t2", cc % 2)], [("mT", cc)])
                mTk = [("mT", cc) for cc in range(8)]

                def proj(srcT, srck, wt, wname, bi):
                    for half in range(2):
                        pso = psf[4 + half]
                        for kc in range(8):
                            P.mm(pso[:], srcT[:, kc, bi * 128:(bi + 1) * 128], wt[:, kc, half * 512:(half + 1) * 512],
                                 kc == 0, kc == 7, srck + wk2(wname), [("psf", 4 + half)])
                        P.stt("dve", xr[:, bi, half * 512:(half + 1) * 512], xr[:, bi, half * 512:(half + 1) * 512], ALPHA,
                              pso[:], ALU.mult, ALU.add, [("psf", 4 + half), (XR, bi)], [(XR, bi)])

                def lnf(lg_, lb_, bi):
                    P.layernorm(xr[:, bi, :], xr[:, bi, :], lg_[:], lb_[:], D, lnt, [(XR, bi)], [(XR, bi)], lnk, "l")

                proj(mT, mTk, wout, "wout", 0)
                for bi in range(4):
                    if bi + 1 < 4:
                        proj(mT, mTk, wout, "wout", bi + 1)
                    lnf(l1g, l1b, bi)
                    xs = bcount % 2
                    P.copy("act", xb16[xs][:], xr[:, bi, :], [(XR, bi)], [("xb16", xs)])
                    pb = psb[bcount % 2]
                    for kc in range(8):
                        P.tr(pb[:, kc * 128:(kc + 1) * 128], xb16[xs][:, kc * 128:(kc + 1) * 128], ident_b[:],
                             [("xb16", xs)], [("psb", bcount % 2)])
                    P.copy("dve", x1T[:, :, bi * 128:(bi + 1) * 128], pb[:].rearrange("p (k t) -> p k t", k=8),
                           [("psb", bcount % 2)], [("x1T", bi)])
                    bcount += 1
                x1k = [("x1T", bi) for bi in range(4)]
                for cc in range(8):
                    pq = psf[cc % 2]
                    for kc in range(8):
                        P.mm(pq[:], wmq[:, kc, cc * 128:(cc + 1) * 128], x1T[:, kc, :], kc == 0, kc == 7,
                             x1k + wk2("wmq"), [("psf", cc % 2)])
                    P.copy("act" if cc % 2 == 0 else "dve", qmT[:, cc, :], pq[:], [("psf", cc % 2)], [("mT", cc)])
                for hm in range(4):
                    for mc in range(2):
                        S = psf[2 + mc]
                        for dc in range(2):
                            P.mm(S[:], kmT[:, 2 * hm + dc, mc * 128:(mc + 1) * 128], qmT[:, 2 * hm + dc, :], dc == 0, dc == 1,
                                 [("mT", 2 * hm + dc)], [("psf", 2 + mc)])
                        P.actf(pm[mc][:], S[:], AF.Exp, [("psf", 2 + mc)], [("pm", mc)], scale=1.0 / 16.0)
                    R = psf[4]
                    for mc in range(2):
                        P.mm(R[:], ones_b[:], pm[mc][:], mc == 0, mc == 1, [("pm", mc)], [("psf", 4)])
                    P.add("dve", lambda e, R=R: e.reciprocal(out=rinv[:], in_=R[:]), [("psf", 4)], ["rinv"])
                    for dc in range(2):
                        Ops = psf[dc]
                        for mc in range(2):
                            P.mm(Ops[:], vmem[:, mc, hm * 256 + dc * 128:hm * 256 + (dc + 1) * 128], pm[mc][:], mc == 0, mc == 1,
                                 [("pm", mc)], [("psf", dc)])
                        P.tt("dve", oT[:, 2 * hm + dc, :], Ops[:], rinv[:], ALU.mult, [("psf", dc), "rinv"], [("x1T", b_) for b_ in range(4)])
                oTk = [("x1T", b_) for b_ in range(4)]
                proj(oT, oTk, wmo, "wmo", 0)
                for bi in range(4):
                    blk = g * 4 + bi
                    if bi + 1 < 4:
                        proj(oT, oTk, wmo, "wmo", bi + 1)
                    lnf(l2g, l2b, bi)
                    P.dma("sp", x2_d[blk * 128:(blk + 1) * 128, :], xr[:, bi, :], "x2st", [(XR, bi)], [("x2_d", blk)])
                    xs = blk % 2
                    P.copy("act", x2b[xs][:], xr[:, bi, :], [(XR, bi)], [("x2b", xs)])
                    for kc in range(8):
                        pt_ = psf[kc // 4]
                        P.tr(pt_[:, (kc % 4) * 128:(kc % 4 + 1) * 128], xr[:, bi, kc * 128:(kc + 1) * 128], ident_f[:],
                             [(XR, bi)], [("psf", kc // 4)])
                    for hh in range(2):
                        P.copy("dve" if hh == 0 else "act", x2Tf[:, hh * 4:(hh + 1) * 4, :],
                               psf[hh][:].rearrange("p (k t) -> p k t", k=4), [("psf", hh)], [("x2Tf", hh)])
                    pl = psf[2]
                    for kc in range(8):
                        P.mm(pl[:, 0:NE], x2Tf[:, kc, :], wr[:, kc, :], kc == 0, kc == 7, [("x2Tf", 0), ("x2Tf", 1), "wr"], [("psf", 2)])
                    P.tt("dve", lg[:], pl[:, 0:NE], br[:], ALU.add, [("psf", 2), "br"], ["lg"])
                    P.add("dve", lambda e: e.max(out=top8[:], in_=lg[:]), ["lg"], ["top8"])
                    P.ts("dve", negm[:], top8[:, 0:1], -1.0, None, ALU.mult, None, ["top8"], ["negm"])
                    P.actf(ex[:], lg[:], AF.Exp, ["lg", "negm"], ["ex"], bias=negm[:, 0:1])
                    P.ts("dve", msk[:], lg[:], top8[:, 3:4], 1.0, ALU.is_ge, ALU.mult, ["lg", "top8"], ["msk"])
                    P.tt("dve", em[:], ex[:], msk[:], ALU.mult, ["ex", "msk"], ["em"])
                    P.add("dve", lambda e: e.tensor_reduce(out=den[:], in_=em[:], axis=AX.X, op=ALU.add), ["em"], ["den"])
                    P.add("dve", lambda e: e.reciprocal(out=rden[:], in_=den[:]), ["den"], ["rden"])
                    P.ts("dve", gwd[:], em[:], rden[:, 0:1], 1.0, ALU.mult, ALU.mult, ["em", "rden"], ["gwd"])
                    pc = psf[3]
                    P.mm(pc[:, 0:NE], ut[:], msk[:], True, True, ["msk"], [("psf", 3)])
                    P.mm(pc[:, NE:2 * NE], ones_f[:], msk[:], True, True, ["msk"], [("psf", 3)])
                    P.tt("dve", dst[:], pc[:, 0:NE], base[:], ALU.add, [("psf", 3), "base"], ["dst"])
                    P.tt("dve", base[:], pc[:, NE:2 * NE], base[:], ALU.add, [("psf", 3), "base"], ["base"])
                    P.ts("dve", pen[:], dst[:], CAP + 0.5, 1.0, ALU.is_gt, ALU.mult, ["dst"], ["pen"])
                    P.stt("dve", dst2[:], dst[:], -1.0, ecap[:], ALU.add, ALU.add, ["dst", "ecap"], ["dst2"])
                    P.tt("dve", tm32[:], trash[:], dst2[:], ALU.subtract, ["dst2", "ecap"], ["tm32"])
                    P.tt("dve", tm32[:], tm32[:], pen[:], ALU.mult, ["tm32", "pen"], ["tm32"])
                    P.tt("dve", dst2[:], dst2[:], tm32[:], ALU.add, ["dst2", "tm32"], ["dst2"])
                    for k in range(4):
                        P.ts("dve", oh[:], lg[:], top8[:, k:k + 1], 1.0, ALU.is_equal, ALU.mult, ["lg", "top8"], ["oh"])
                        P.tt("dve", tm32[:], oh[:], dst2[:], ALU.mult, ["oh", "dst2"], ["tm32"])
                        P.add("dve", (lambda k: lambda e: e.tensor_reduce(out=d4f[:, k:k + 1], in_=tm32[:], axis=AX.X, op=ALU.add))(k),
                              ["tm32"], ["d4f"])
                        P.tt("dve", tm32[:], oh[:], gwd[:], ALU.mult, ["oh", "gwd", "d4f"], ["tm32"])
                        P.add("dve", (lambda k, blk: lambda e: e.tensor_reduce(out=gw4[:, blk, k:k + 1], in_=tm32[:], axis=AX.X, op=ALU.add))(k, blk),
                              ["tm32"], [("gw4", blk)])
                    P.copy("dve", dest4[:, blk, :], d4f[:], ["d4f"], [("dest4", blk)])
                    for k in range(4):
                        si = (blk % 2) * 4 + k
                        P.copy("dve", sidx[si][:], d4f[:, k:k + 1], ["d4f"], [("sidx", si)])
                        P.add("pool", (lambda si, xs: lambda e: e.indirect_dma_start(
                            out=xg_d, out_offset=bass.IndirectOffsetOnAxis(ap=sidx[si][:, :], axis=0),
                            in_=x2b[xs][:, :], in_offset=None))(si, xs),
                            [("sidx", si), ("x2b", xs)], [("xg_d", blk, k)], dma="scat%d" % xs)
            if debug:
                P.dma("sp", cnt_d, base[:], "cntd", ["base"], ["cnt_d"])
            P.emit()
        ph45.close()
        if stop_after < 6:
            return nc, ins

        NT = CAP // 128
        cgs = []
        c0 = 0
        while c0 < CAP:
            cgs.append((c0, min(512, CAP - c0)))
            c0 += 512
        with ExitStack() as ph:
            def psb_(name, shape, dt):
                return sb(name, shape, dt, ph)
            wgu = [psb_("wgu%d" % i, [128, 8, 2 * D], BF16) for i in range(2)]
            wdn = [psb_("wdn%d" % i, [128, 8, D], BF16) for i in range(2)]
            bgu = [psb_("bgu%d" % i, [128, 16], F32) for i in range(2)]
            bdb = [psb_("bdb%d" % i, [128, D], F32) for i in range(2)]
            xgt = [psb_("xgt%d" % i, [128, D], BF16) for i in range(3)]
            xgT = psb_("xgT", [128, 8, CAP], BF16)
            actT = psb_("actT", [128, 8, CAP], BF16)
            gq = [psb_("gq%d" % i, [128, 512], F32) for i in range(2)]
            sq = [psb_("sq%d" % i, [128, 512], F32) for i in range(2)]
            uq = [psb_("uq%d" % i, [128, 512], F32) for i in range(2)]
            ys = [psb_("ys%d" % i, [128, D], BF16) for i in range(2)]
            zt = psb_("zt", [128, D], BF16)
            P.memset("pool", zt[:], 0.0, ["zt"])
            P.dma("sp", ye_d[NSLOT:NSLOT + 128, :], zt[:], "zt", ["zt"], ["ye_trash"])
            ne6 = NE if stop_after >= 6 else 2

            def exp_load(e):
                s_ = e % 2
                for i in range(4):
                    P.dma("pool", wgu[s_][:, :, i * 512:(i + 1) * 512],
                          w_gu[e][:, i * 512:(i + 1) * 512].rearrange("(kc p) c -> p kc c", p=128), "wgu%d_%d" % (s_, i), [], [("wgu", s_, i)])
                for i in range(2):
                    P.dma("pool", wdn[s_][:, :, i * 512:(i + 1) * 512],
                          w_d[e][:, i * 512:(i + 1) * 512].rearrange("(kc p) c -> p kc c", p=128), "wdn%d_%d" % (s_, i), [], [("wdn", s_, i)])
                P.dma("sp", bdb[s_][:], b_d[e:e + 1, :].to_broadcast([128, D]), "bdb%d" % s_, [], [("bdb", s_)])
                P.dma("sp", bgu[s_][:], b_gu[e], "bgu%d" % s_, [], [("bgu", s_)])

            exp_load(0)
            tcount = 0
            ecount = 0
            ycount = 0
            for e in range(ne6):
                s_ = e % 2
                if e + 1 < ne6:
                    exp_load(e + 1)
                for t in range(NT):
                    xs = tcount % 3
                    P.dma("sp", xgt[xs][:], xg_d[e * CAP + t * 128:e * CAP + (t + 1) * 128, :], "xgt%d" % xs, [], [("xgt", xs)])
                    pb = psb[tcount % 2]
                    for kc in range(8):
                        P.tr(pb[:, kc * 128:(kc + 1) * 128], xgt[xs][:, kc * 128:(kc + 1) * 128], ident_b[:], [("xgt", xs)], [("psb", tcount % 2)])
                    P.copy("dve" if t % 2 == 0 else "act", xgT[:, :, t * 128:(t + 1) * 128], pb[:].rearrange("p (k t) -> p k t", k=8),
                           [("psb", tcount % 2)], [("xgT", t)])
                    tcount += 1
                wguk = [("wgu", s_, i) for i in range(4)]
                wdnk = [("wdn", s_, i) for i in range(2)]
                for fc in range(8):
                    for (c0, ncol) in cgs:
                        b0 = ecount % 2
                        ecount += 1
                        G = psf[b0]; U = psf[2 + b0]
                        xk = [("xgT", t) for t in range(c0 // 128, (c0 + ncol) // 128)]
                        for kc in range(8):
                            P.mm(G[:, 0:ncol], wgu[s_][:, kc, fc * 128:(fc + 1) * 128], xgT[:, kc, c0:c0 + ncol], kc == 0, kc == 7,
                                 xk + wguk, [("psf", b0)])
                        for kc in range(8):
                            P.mm(U[:, 0:ncol], wgu[s_][:, kc, D + fc * 128:D + (fc + 1) * 128], xgT[:, kc, c0:c0 + ncol], kc == 0, kc == 7,
                                 xk + wguk, [("psf", 2 + b0)])
                        P.ts("dve", gq[b0][:, 0:ncol], G[:, 0:ncol], bgu[s_][:, fc:fc + 1], 7.0, ALU.add, ALU.min,
                             [("psf", b0), ("bgu", s_)], [("gq", b0)])
                        P.actf(sq[b0][:, 0:ncol], gq[b0][:, 0:ncol], AF.Sigmoid, [("gq", b0)], [("sq", b0)], scale=1.702)
                        P.ts("dve", uq[b0][:, 0:ncol], U[:, 0:ncol], bgu[s_][:, 8 + fc:9 + fc], 7.0, ALU.add, ALU.min,
                             [("psf", 2 + b0), ("bgu", s_)], [("uq", b0)])
                        P.ts("dve", uq[b0][:, 0:ncol], uq[b0][:, 0:ncol], -7.0, 1.0, ALU.max, ALU.add, [("uq", b0)], [("uq", b0)])
                        P.tt("pool", gq[b0][:, 0:ncol], gq[b0][:, 0:ncol], sq[b0][:, 0:ncol], ALU.mult, [("gq", b0), ("sq", b0)], [("gq", b0)])
                        P.tt("dve", actT[:, fc, c0:c0 + ncol], uq[b0][:, 0:ncol], gq[b0][:, 0:ncol], ALU.mult,
                             [("uq", b0), ("gq", b0)], [("actT", fc)])
                ak = [("actT", fc) for fc in range(8)]
                for t in range(NT):
                    ysl = ycount % 2
                    ycount += 1
                    for half in range(2):
                        Y = psf[4 + half]
                        for fc in range(8):
                            P.mm(Y[:], actT[:, fc, t * 128:(t + 1) * 128], wdn[s_][:, fc, half * 512:(half + 1) * 512], fc == 0, fc == 7,
                                 ak + wdnk, [("psf", 4 + half)])
                        P.tt("dve", ys[ysl][:, half * 512:(half + 1) * 512], Y[:], bdb[s_][:, half * 512:(half + 1) * 512], ALU.add,
                             [("psf", 4 + half), ("bdb", s_)], [("ys", ysl)])
                    P.dma("sp", ye_d[e * CAP + t * 128:e * CAP + (t + 1) * 128, :], ys[ysl][:], "ys%d" % ysl, [("ys", ysl)], [("ye_d", e, t)])
            P.emit()
        if stop_after < 7:
            return nc, ins

        with ExitStack() as ph:
            def psb_(name, shape, dt):
                return sb(name, shape, dt, ph)
            l3g = psb_("l3g", [128, D], F32); l3b = psb_("l3b", [128, D], F32)
            P.dma("sp", l3g[:], ln3_g, "c0", [], ["lnp0"])
            P.dma("sp", l3b[:], ln3_b, "c1", [], ["lnp1"])
            yg = [[psb_("yg%d_%d" % (i, k), [128, D], BF16) for k in range(4)] for i in range(3)]
            gidx = [[psb_("gidx%d_%d" % (i, k), [128, 1], I32) for k in range(4)] for i in range(3)]
            x2t = [psb_("x2t%d" % i, [128, D], F32) for i in range(3)]
            for blk in range(NOB):
                sl = blk % 3
                P.dma("sp", x2t[sl][:], x2_d[blk * 128:(blk + 1) * 128, :], "x2t%d" % sl, [], [("x2t", sl)])
                for k in range(4):
                    P.copy("dve", gidx[sl][k][:], dest4[:, blk, k:k + 1], [], [("gidx", sl, k)])
                    P.add("pool", (lambda sl, k: lambda e: e.indirect_dma_start(
                        out=yg[sl][k][:, :], out_offset=None, in_=ye_d,
                        in_offset=bass.IndirectOffsetOnAxis(ap=gidx[sl][k][:, :], axis=0)))(sl, k),
                        [("gidx", sl, k)], [("yg", sl, k)], dma="yg%d_%d" % (sl, k))
                P.actf(x2t[sl][:], x2t[sl][:], AF.Copy, [("x2t", sl)], [("x2t", sl)], scale=ALPHA)
                for k in range(4):
                    P.stt("dve", x2t[sl][:], yg[sl][k][:], gw4[:, blk, k:k + 1], x2t[sl][:], ALU.mult, ALU.add,
                          [("yg", sl, k), ("x2t", sl)], [("x2t", sl)])
                P.layernorm(x2t[sl][:], x2t[sl][:], l3g[:], l3b[:], D, lnt, [("x2t", sl)], [("x2t", sl)], ["lnp0", "lnp1"], "l")
                P.dma("sp", out_d[blk * 128:(blk + 1) * 128, :], x2t[sl][:], "out%d" % sl, [("x2t", sl)], [("out_d", blk)])
            P.emit()
    return nc, ins


_CACHE = {}


def kernel(**inputs):
    inp = {k: np.asarray(v) for k, v in inputs.items()}
    if "nc" not in _CACHE:
        _CACHE["nc"] = build_program(debug=False)
    nc, ins = _CACHE["nc"]
    rep = lambda v, n=128: np.ascontiguousarray(np.broadcast_to(np.asarray(v, np.float32).reshape(1, -1), (n, np.asarray(v).size)))
    shared = {}
    shared["lnin_g"] = rep(inp["ln_in_g"]); shared["lnin_b"] = rep(inp["ln_in_b"])
    shared["w_in"] = np.ascontiguousarray(inp["w_in"][0]); shared["bfor"] = rep(inp["b_forget"][0])
    shared["lnv_g"] = rep(inp["ln_v_g"][0]); shared["lnv_b"] = rep(inp["ln_v_b"][0])
    shared["wspT"] = np.ascontiguousarray(np.transpose(inp["w_spatial"][0], (0, 2, 1)))
    bs = inp["b_spatial"][0]
    shared["bspf"] = np.ascontiguousarray(np.repeat(bs.T[:, :, None], 64, axis=2).reshape(128, 512)).astype(np.float32)
    shared["w_ba"] = np.ascontiguousarray(inp["w_branch_a"][0]); shared["w_bb"] = np.ascontiguousarray(inp["w_branch_b"][0])
    shared["w_out"] = np.ascontiguousarray(inp["w_out"][0])
    shared["ln1_g"] = rep(inp["ln1_g"][0]); shared["ln1_b"] = rep(inp["ln1_b"][0])
    shared["lnm_g"] = rep(inp["ln_mem_g"][0]); shared["lnm_b"] = rep(inp["ln_mem_b"][0])
    shared["w_mq"] = np.ascontiguousarray(inp["w_mq"][0]); shared["w_mkv"] = np.ascontiguousarray(inp["w_mkv"][0])
    shared["w_mo"] = np.ascontiguousarray(inp["w_mo"][0])
    shared["ln2_g"] = rep(inp["ln2_g"][0]); shared["ln2_b"] = rep(inp["ln2_b"][0])
    shared["w_r"] = np.ascontiguousarray(inp["w_router"][0]); shared["b_r"] = rep(inp["b_router"][0])
    shared["w_gu"] = np.ascontiguousarray(inp["w_gate_up"][0])
    shared["b_gu"] = np.ascontiguousarray(inp["b_gate_up"][0].reshape(NE, 16, 128).transpose(0, 2, 1))
    shared["w_d"] = np.ascontiguousarray(inp["w_down"][0]); shared["b_d"] = np.ascontiguousarray(inp["b_down"][0])
    shared["ln3_g"] = rep(inp["ln3_g"][0]); shared["ln3_b"] = rep(inp["ln3_b"][0])
    consts = [_consts(0), _consts(1)]
    in_maps = []
    for c in range(8):
        b, p = c // 2, c % 2
        x = inp["x"][b]
        d = dict(shared)
        d["x_all"] = np.ascontiguousarray(x)
        d["x_own"] = np.ascontiguousarray(x.reshape(NB, 128, D)[p::2].reshape(NOB * 128, D))
        d["mem"] = np.ascontiguousarray(inp["mem"][b])
        d.update(consts[p])
        in_maps.append({k: v for k, v in d.items() if k in ins})
    res = run_bass_kernel_spmd(nc, in_maps, core_ids=list(range(8)))
    out = np.empty((4, SEQ, D), np.float32)
    for c in range(8):
        b, p = c // 2, c % 2
        out[b].reshape(NB, 128, D)[p::2] = np.asarray(res.results[c]["out"]).reshape(NOB, 128, D)
    return out
```
